# Optimizing a Trainium2 kernel written in Bass

```python
import jax, jax.numpy as jnp
from jax import lax
import numpy as np

D_MODEL = 1024
BATCH = 8
SEQ = 4096
DEPTH = 1

D_MIX = 2 * D_MODEL
D_SSD = D_MIX // 2
D_SC = D_MIX - D_SSD
SSD_HEADDIM = 64
SSD_HEADS = D_SSD // SSD_HEADDIM
SSD_GROUPS = 4
SSD_HPG = SSD_HEADS // SSD_GROUPS
SSD_STATE = 128
SSD_CONV = 4
D_XBC = D_SSD + 2 * SSD_GROUPS * SSD_STATE
CHUNK = 128
SC_GROUPS = 16
SC_CONV = 3
N_META = 16
META_PAD = CHUNK - N_META
IN_COLS = D_SSD + D_XBC + SSD_HEADS + 3 * D_SC
N_EXPERT_GROUPS = 4
EXPERTS_PER_GROUP = 4
N_EXPERTS = N_EXPERT_GROUPS * EXPERTS_PER_GROUP
TOP_K = 2
D_EXPERT = 512
EPS = 1e-6

kernel_name = "hymba_ssd_shortconv_hier_moe_block"


def rmsnorm(x, g):
    xf = x.astype(jnp.float32)
    xf = xf * lax.rsqrt(jnp.mean(xf * xf, axis=-1, keepdims=True) + EPS)
    return (xf * g.astype(jnp.float32)).astype(x.dtype)


def group_rmsnorm(x, g, n_groups):
    shp = x.shape
    xf = x.astype(jnp.float32).reshape(shp[:-1] + (n_groups, shp[-1] // n_groups))
    xf = xf * lax.rsqrt(jnp.mean(xf * xf, axis=-1, keepdims=True) + EPS)
    return (xf.reshape(shp) * g.astype(jnp.float32)).astype(x.dtype)


def causal_dwconv(x, w):
    K = w.shape[0]
    L = x.shape[1]
    xp = jnp.pad(x, ((0, 0), (K - 1, 0), (0, 0)))
    y = xp[:, 0:L] * w[0]
    for k in range(1, K):
        y = y + xp[:, k:k + L] * w[k]
    return y


def ssd_chunked(xs, dt, A, Bm, Cm):
    dtype = xs.dtype
    b, Lp = xs.shape[0], xs.shape[1]
    nc = Lp // CHUNK
    x = xs.reshape(b, nc, CHUNK, SSD_GROUPS, SSD_HPG, SSD_HEADDIM)
    dtc = dt.reshape(b, nc, CHUNK, SSD_GROUPS, SSD_HPG)
    Bc = Bm.reshape(b, nc, CHUNK, SSD_GROUPS, SSD_STATE)
    Cc = Cm.reshape(b, nc, CHUNK, SSD_GROUPS, SSD_STATE)

    dA = jnp.moveaxis(dtc * A.reshape(SSD_GROUPS, SSD_HPG), 2, -1)
    A_cum = jnp.cumsum(dA, axis=-1)
    xdt = x * dtc.astype(dtype)[..., None]

    causal = jnp.tril(jnp.ones((CHUNK, CHUNK), dtype=bool))
    seg = A_cum[..., :, None] - A_cum[..., None, :]
    Lmat = jnp.exp(jnp.where(causal, seg, -jnp.inf)).astype(dtype)
    CB = jnp.einsum('bcqgn,bckgn->bcgqk', Cc, Bc)
    y_diag = jnp.einsum('bcgqk,bcgrqk,bckgrp->bcqgrp', CB, Lmat, xdt)

    decay_states = jnp.exp(A_cum[..., -1:] - A_cum).astype(dtype)
    states = jnp.einsum('bckgn,bcgrk,bckgrp->bcgrpn', Bc, decay_states, xdt)
    chunk_decay = jnp.exp(A_cum[..., -1]).astype(dtype)

    def step(s, inp):
        st, dec = inp
        return dec[..., None, None] * s + st, s

    s0 = jnp.zeros((b, SSD_GROUPS, SSD_HPG, SSD_HEADDIM, SSD_STATE), dtype)
    _, prev = lax.scan(step, s0, (jnp.moveaxis(states, 1, 0), jnp.moveaxis(chunk_decay, 1, 0)))
    prev = jnp.moveaxis(prev, 0, 1)

    decay_out = jnp.exp(A_cum).astype(dtype)
    y_off = jnp.einsum('bcqgn,bcgrpn,bcgrq->bcqgrp', Cc, prev, decay_out)
    return (y_diag + y_off).reshape(b, Lp, SSD_HEADS, SSD_HEADDIM)


def ssd_branch(z, xbc, dt_raw, conv_w, conv_b, dt_bias, A_log, D_skip, norm_g):
    b, L, _ = xbc.shape
    xbc = jax.nn.silu(causal_dwconv(xbc, conv_w) + conv_b)
    xs = xbc[..., :D_SSD].reshape(b, L, SSD_HEADS, SSD_HEADDIM)
    Bm = xbc[..., D_SSD:D_SSD + SSD_GROUPS * SSD_STATE].reshape(b, L, SSD_GROUPS, SSD_STATE)
    Cm = xbc[..., D_SSD + SSD_GROUPS * SSD_STATE:].reshape(b, L, SSD_GROUPS, SSD_STATE)
    dt = jax.nn.softplus((dt_raw + dt_bias).astype(jnp.float32))
    A = -jnp.exp(A_log.astype(jnp.float32))

    def front_pad(t):
        return jnp.pad(t, ((0, 0), (META_PAD, 0)) + ((0, 0),) * (t.ndim - 2))

    y = ssd_chunked(front_pad(xs), front_pad(dt), A, front_pad(Bm), front_pad(Cm))[:, META_PAD:]
    y = y + D_skip[:, None] * xs
    y = y.reshape(b, L, D_SSD)
    return group_rmsnorm(y * jax.nn.silu(z), norm_g, SSD_GROUPS)


def short_conv_branch(gate_b, gate_c, h, conv_w, norm_g):
    v = causal_dwconv(gate_c * h, conv_w)
    return group_rmsnorm(gate_b * v, norm_g, SC_GROUPS)


def hier_moe(u, wg_r, bg_r, we_r, be_r, w_gate, w_up, w_down):
    b, L, d = u.shape
    t = u.reshape(b * L, d)
    p_group = jax.nn.softmax((t @ wg_r + bg_r).astype(jnp.float32), axis=-1)
    g_val, g_idx = lax.top_k(p_group, 1)
    e_logits = (t @ we_r + be_r).astype(jnp.float32).reshape(-1, N_EXPERT_GROUPS, EXPERTS_PER_GROUP)
    sel = jnp.take_along_axis(e_logits, g_idx[:, :, None], axis=1)[:, 0]
    top_v, top_i = lax.top_k(sel, TOP_K)
    top_w = jax.nn.softmax(top_v, axis=-1)
    in_w = jnp.einsum('tk,tke->te', top_w, jax.nn.one_hot(top_i, EXPERTS_PER_GROUP, dtype=jnp.float32))
    grp_w = jax.nn.one_hot(g_idx[:, 0], N_EXPERT_GROUPS, dtype=jnp.float32) * g_val
    combine = (grp_w[:, :, None] * in_w[:, None, :]).reshape(-1, N_EXPERTS).astype(u.dtype)
    out = jnp.zeros_like(t)
    for e in range(N_EXPERTS):
        h = jax.nn.silu(t @ w_gate[e]) * (t @ w_up[e])
        out = out + combine[:, e:e + 1] * (h @ w_down[e])
    return out.reshape(b, L, d)


def setup_inputs(seed: int = 0) -> dict:
    key = jax.random.key(seed)
    ks = jax.random.split(key, 24)
    f32 = jnp.float32
    nrm = lambda k, shp, s: (jax.random.normal(k, shp, f32) * s)
    dt0 = jnp.exp(jax.random.uniform(ks[6], (DEPTH, SSD_HEADS), f32, np.log(1e-3), np.log(1e-1)))
    return {
        "x": jax.random.normal(ks[0], (BATCH, SEQ, D_MODEL), f32),
        "meta_tokens": nrm(ks[1], (N_META, D_MODEL), 1.0),
        "norm_mix": 1.0 + nrm(ks[2], (DEPTH, D_MODEL), 0.02),
        "w_in": nrm(ks[3], (DEPTH, D_MODEL, IN_COLS), D_MODEL ** -0.5),
        "ssd_conv_w": nrm(ks[4], (DEPTH, SSD_CONV, D_XBC), SSD_CONV ** -0.5),
        "ssd_conv_b": nrm(ks[5], (DEPTH, D_XBC), 0.02),
        "dt_bias": dt0 + jnp.log(-jnp.expm1(-dt0)),
        "A_log": jnp.log(jax.random.uniform(ks[7], (DEPTH, SSD_HEADS), f32, 1.0, 16.0)),
        "D_skip": 1.0 + nrm(ks[8], (DEPTH, SSD_HEADS), 0.1),
        "ssd_norm": 1.0 + nrm(ks[9], (DEPTH, D_SSD), 0.02),
        "sc_conv_w": nrm(ks[10], (DEPTH, SC_CONV, D_SC), SC_CONV ** -0.5),
        "sc_norm": 1.0 + nrm(ks[11], (DEPTH, D_SC), 0.02),
        "w_out": nrm(ks[12], (DEPTH, D_MIX, D_MODEL), D_MIX ** -0.5),
        "norm_ffn": 1.0 + nrm(ks[13], (DEPTH, D_MODEL), 0.02),
        "router_group_w": nrm(ks[14], (DEPTH, D_MODEL, N_EXPERT_GROUPS), D_MODEL ** -0.5),
        "router_group_b": nrm(ks[15], (DEPTH, N_EXPERT_GROUPS), 0.01),
        "router_expert_w": nrm(ks[16], (DEPTH, D_MODEL, N_EXPERTS), D_MODEL ** -0.5),
        "router_expert_b": nrm(ks[17], (DEPTH, N_EXPERTS), 0.01),
        "expert_w_gate": nrm(ks[18], (DEPTH, N_EXPERTS, D_MODEL, D_EXPERT), D_MODEL ** -0.5),
        "expert_w_up": nrm(ks[19], (DEPTH, N_EXPERTS, D_MODEL, D_EXPERT), D_MODEL ** -0.5),
        "expert_w_down": nrm(ks[20], (DEPTH, N_EXPERTS, D_EXPERT, D_MODEL), D_EXPERT ** -0.5),
        "norm_final": 1.0 + nrm(ks[21], (D_MODEL,), 0.02),
    }


def reference(x, meta_tokens, norm_mix, w_in, ssd_conv_w, ssd_conv_b, dt_bias, A_log, D_skip,
              ssd_norm, sc_conv_w, sc_norm, w_out, norm_ffn, router_group_w, router_group_b,
              router_expert_w, router_expert_b, expert_w_gate, expert_w_up, expert_w_down,
              norm_final):
    b = x.shape[0]
    meta = jnp.broadcast_to(meta_tokens.astype(x.dtype)[None], (b, N_META, D_MODEL))
    h = jnp.concatenate([meta, x], axis=1)
    o_z = D_SSD
    o_xbc = o_z + D_XBC
    o_dt = o_xbc + SSD_HEADS
    o_b = o_dt + D_SC
    o_c = o_b + D_SC
    for l in range(DEPTH):
        u = rmsnorm(h, norm_mix[l])
        proj = u @ w_in[l]
        y_ssd = ssd_branch(proj[..., :o_z], proj[..., o_z:o_xbc], proj[..., o_xbc:o_dt],
                           ssd_conv_w[l], ssd_conv_b[l], dt_bias[l], A_log[l], D_skip[l], ssd_norm[l])
        y_sc = short_conv_branch(proj[..., o_dt:o_b], proj[..., o_b:o_c], proj[..., o_c:],
                                 sc_conv_w[l], sc_norm[l])
        h = h + jnp.concatenate([y_ssd, y_sc], axis=-1) @ w_out[l]
        u2 = rmsnorm(h, norm_ffn[l])
        h = h + hier_moe(u2, router_group_w[l], router_group_b[l], router_expert_w[l],
                         router_expert_b[l], expert_w_gate[l], expert_w_up[l], expert_w_down[l])
    return rmsnorm(h, norm_final)[:, N_META:]
```

```python
from contextlib import ExitStack

import numpy as np
import concourse.bass as bass
import concourse.mybir as mybir
from concourse.bass_utils import run_bass_kernel_spmd

AF = mybir.ActivationFunctionType
ALU = mybir.AluOpType
AX = mybir.AxisListType
F32 = mybir.dt.float32
BF16 = mybir.dt.bfloat16

D = 1024
SEQ = 4096
NCORES = 8
EPS = 1e-6
NPIECE = 66
RING_F = 2
RING_M = 4
DGE_SCRATCH = 16384
RING = RING_F + RING_M
BIG = 30000.0
FSPEED = 1.1
HOTGAP = 0
XLAT = 1.2
LOOKAHEAD = 0
PECYC = 2000.0
MAXSKEW = 0.12
DEBUG = None

ENGS = ["pe", "act", "dve", "pool", "sp"]
SEM_LIMIT = 20000


class _Op:
    __slots__ = ("eng", "fn", "deps", "signal", "dma", "dma_val", "epoch", "cnt", "waits")

    def __init__(self, eng, fn, dma):
        self.eng = eng
        self.fn = fn
        self.dma = dma
        self.deps = []
        self.signal = False
        self.dma_val = 0
        self.epoch = 0
        self.cnt = 0
        self.waits = {}


class Sched:
    def __init__(self):
        self.q = {e: [] for e in ENGS}
        self.last_w = {}
        self.readers = {}
        self.dma_count = {}

    def add(self, eng, fn, reads=(), writes=(), dma=None):
        op = _Op(eng, fn, dma)
        deps = []
        for r in reads:
            w = self.last_w.get(r)
            if w is not None:
                deps.append(w)
        for b in writes:
            lw = self.last_w.get(b)
            if lw is not None and (lw.eng != eng or lw.dma or dma or eng != "pe"):
                deps.append(lw)
            for rd in self.readers.get(b, ()):
                if rd.eng != eng or rd.dma or dma:
                    deps.append(rd)
        op.deps = deps
        for r in reads:
            self.readers.setdefault(r, []).append(op)
        for b in writes:
            self.last_w[b] = op
            self.readers[b] = []
        if dma is not None:
            self.dma_count[dma] = self.dma_count.get(dma, 0) + 16
            op.dma_val = self.dma_count[dma]
        self.q[eng].append(op)
        return op

    def finalize(self):
        for e in ENGS:
            for op in self.q[e]:
                for d in op.deps:
                    if d.dma is None:
                        d.signal = True
        self.sem_keys = set()
        for e in ENGS:
            cnt = 0
            epoch = 0
            for op in self.q[e]:
                if op.dma is None and op.signal:
                    cnt += 1
                    if cnt > SEM_LIMIT:
                        epoch += 1
                        cnt = 1
                    op.epoch = epoch
                    op.cnt = cnt
                    self.sem_keys.add(("c", e, epoch))
                if op.dma is not None:
                    self.sem_keys.add(("d", op.dma))
        for e in ENGS:
            waited = {}
            for op in self.q[e]:
                w = {}
                for d in op.deps:
                    if d.dma is not None:
                        key, val = ("d", d.dma), d.dma_val
                    else:
                        key, val = ("c", d.eng, d.epoch), d.cnt
                    if waited.get(key, 0) >= val:
                        continue
                    if w.get(key, 0) < val:
                        w[key] = val
                for k, v in w.items():
                    waited[k] = v
                op.waits = w

    def emit(self, nc):
        self.finalize()
        with ExitStack() as es:
            sems = {}
            for i, k in enumerate(sorted(self.sem_keys, key=str)):
                sems[k] = es.enter_context(nc.semaphore("s%d" % i))
            block = es.enter_context(nc.Block())

            def run(e, eng):
                for op in self.q[e]:
                    for k, v in op.waits.items():
                        eng.wait_ge(sems[k], v)
                    if op.fn is None:
                        continue
                    inst = op.fn(eng)
                    if op.dma is not None:
                        inst.then_inc(sems[("d", op.dma)], 16)
                    elif op.signal:
                        inst.then_inc(sems[("c", e, op.epoch)], 1)

            @block.tensor
            def _(eng):
                run("pe", eng)

            @block.scalar
            def _(eng):
                run("act", eng)

            @block.vector
            def _(eng):
                run("dve", eng)

            @block.gpsimd
            def _(eng):
                run("pool", eng)

            @block.sync
            def _(eng):
                run("sp", eng)


def build_program():
    nc = bass.Bass("TRN2", target_bir_lowering=False, dynamic_dma_scratch_size=DGE_SCRATCH)
    SS = [Sched()]
    REC = {"list": None}
    STREAM = {"cur": "F"}

    def emit_op(eng, fn, reads=(), writes=(), dma=None, cost=0.3):
        if REC["list"] is not None:
            REC["list"].append((eng, fn, tuple(reads), tuple(writes), dma, cost))
        else:
            SS[0].add(eng, fn, reads, writes, dma)

    def marker(kind, name):
        if REC["list"] is not None:
            REC["list"].append((kind, name))

    def din(name, shape, dt=F32):
        return nc.dram_tensor(name, shape, dt, kind="ExternalInput").ap()

    x_d = din("x", [SEQ, D])
    meta_d = din("meta", [16, D])
    wall_d = din("wall", [NPIECE, 128, 4096])
    wsm_d = din("wsmall", [128, 8 * 36])
    vbc_d = din("vbc", [128, 1092])
    vpp_d = din("vpp", [128, 136])
    cst_d = din("cst", [128, 641])
    out_d = nc.dram_tensor("out", [SEQ, D], F32, kind="ExternalOutput").ap()
    scr_d = nc.dram_tensor("scr", [NPIECE, 128, 4096], BF16, kind="Internal").ap()
    dbg_d = None
    if DEBUG is not None:
        dbg_d = nc.dram_tensor("dbg", [8, 128, 4096], F32, kind="ExternalOutput").ap()

    es = ExitStack()
    with es:
        def sb(name, shape, dt=F32):
            return es.enter_context(nc.sbuf_tensor("sb_" + name, shape, dt))

        ring = [sb("ring%d" % i, [128, 4096], BF16) for i in range(RING)]
        hres = sb("hres", [128, 4, 1024])
        xst = [sb("xst%d" % i, [128, 1024]) for i in range(1)]
        uT = sb("uT", [128, 8, 512], BF16)
        u2T = sb("u2T", [128, 8, 512], BF16)
        ub = sb("ub", [128, 1024], BF16)
        ub2 = sb("ub2", [128, 1024], BF16)
        bsb = sb("bsb", [128, 512])
        dgF = sb("dgF", [128, 128])
        dgM = sb("dgM", [128, 128])
        sm2 = sb("sm2", [128, 32])
        sz = sb("sz", [128, 4, 1024], BF16)
        xbcT = sb("xbcT", [128, 16, 512], BF16)
        ycatT = sb("ycatT", [128, 16, 512], BF16)
        dtraw = sb("dtraw", [128, 4, 16])
        raw = [sb("raw%d" % i, [128, 515]) for i in range(1)]
        cacc = [sb("cacc%d" % i, [128, 512]) for i in range(1)]
        halo_x = sb("halo_x", [128, 16, 3])
        halo_s = sb("halo_s", [128, 8, 2])
        csb = sb("csb", [128, 512])
        rawsc = sb("rawsc", [128, 514])
        vsc = sb("vsc", [128, 512])
        xtok = [sb("xtok%d" % i, [128, 1024], BF16) for i in range(2)]
        btok = [sb("btok%d" % i, [128, 512], BF16) for i in range(2)]
        xdt = sb("xdt", [128, 1024], BF16)
        xdd = sb("xdd", [128, 1024], BF16)
        LTb = [sb("LT%d" % i, [128, 512], BF16) for i in range(2)]
        MTb = [sb("MT%d" % i, [128, 512], BF16) for i in range(4)]
        state = sb("state", [128, 1024])
        state_bf = sb("state_bf", [128, 1024], BF16)
        y1 = sb("y1", [128, 1024])
        xD = sb("xD", [128, 1024])
        yn = sb("yn", [128, 1024], BF16)
        sm = sb("sm", [128, 256])
        acT = sb("acT", [16, 128])
        nacT = sb("nacT", [16, 128])
        rt_all = sb("rt", [128, 640])
        combT = sb("combT", [16, 512])
        cb_sb = sb("cb_sb", [128, 512])
        s_sb = [sb("s_sb%d" % i, [128, 512]) for i in range(1)]
        t_sb = [sb("t_sb%d" % i, [128, 512]) for i in range(2)]
        hT = [sb("hT%d" % i, [128, 4, 512], BF16) for i in range(2)]
        cst = sb("cst", [128, 641])
        vbc = sb("vbc", [128, 1092])
        vpp = sb("vpp", [128, 136])
        wsm_bf = sb("wsm_bf", [128, 288], BF16)
        cbf = sb("cbf", [128, 384], BF16)
        A_bc = sb("A_bc", [128, 16])
        pbank = [es.enter_context(nc.psum_tensor("pb%d" % i, [128, 512], F32)) for i in range(8)]

        I_f = cst[:, 0:128]
        TRI = cst[:, 128:256]
        ONES = cst[:, 256:384]
        PADM = cst[:, 640:641]
        I_b = cbf[:, 0:128]
        MASKN = cbf[:, 128:256]
        BLK = cbf[:, 256:384]
        GFIN = vbc[:, 0:1024]
        DTB = vbc[:, 1024:1040]
        DSK = vbc[:, 1056:1072]
        RBIAS = vbc[:, 1072:1092]
        GMIX = vpp[:, 0:8]
        GFFN = vpp[:, 8:16]
        GSSD = vpp[:, 16:24]
        GSC = vpp[:, 24:32]
        CW = vpp[:, 32:96]
        CB = vpp[:, 96:112]
        SCW = vpp[:, 112:136]

        def selv(h):
            return cst[0:16, h:h + 1].to_broadcast([16, 128])


        BANKS = {"F": [0, 1, 2, 3], "M": [4, 5, 6, 7]}
        held = set()
        psc = {"F": 0, "M": 0}

        def newbank():
            st = STREAM["cur"]
            bl = BANKS[st]
            for _ in range(len(bl)):
                i = bl[psc[st] % len(bl)]
                psc[st] += 1
                if i not in held:
                    held.add(i)
                    return i
            raise RuntimeError("no free PSUM bank for stream " + st)

        def relb(*bs):
            for b_ in bs:
                held.discard(b_)

        def fsz(ap):
            n = 1
            for d_ in ap.shape[1:]:
                n *= d_
            return n

        def ecost(eng, out):
            n = fsz(out)
            if eng == "dve":
                return 0.07 + n / 960.0
            if eng == "pool":
                return 0.3 + n / 550.0
            return 0.22 + n / 1200.0

        def mm(out, lhsT, rhs, start, stop, reads, writes):
            passes = 4 if lhsT.dtype == F32 else 1
            c = max(fsz(out) * passes / PECYC, 0.055)
            emit_op("pe", lambda e: e.matmul(out, lhsT=lhsT, rhs=rhs, start=start, stop=stop), reads, writes, cost=c)

        def act(out, in_, func, reads, writes, bias=None, scale=None, accum=None):
            kw = {}
            if bias is not None:
                kw["bias"] = bias
            if scale is not None:
                kw["scale"] = scale
            if accum is not None:
                kw["accum_out"] = accum
            emit_op("act", lambda e: e.activation(out=out, in_=in_, func=func, **kw), reads, writes,
                    cost=0.22 + fsz(in_) / 1200.0)

        def tt(eng, out, in0, in1, op, reads, writes):
            emit_op(eng, lambda e: e.tensor_tensor(out=out, in0=in0, in1=in1, op=op), reads, writes, cost=ecost(eng, out))

        def ts(eng, out, in0, s1, s2, op0, op1, reads, writes):
            if s2 is None:
                emit_op(eng, lambda e: e.tensor_scalar(out=out, in0=in0, scalar1=s1, scalar2=None, op0=op0), reads, writes,
                        cost=ecost(eng, out))
            else:
                emit_op(eng, lambda e: e.tensor_scalar(out=out, in0=in0, scalar1=s1, scalar2=s2, op0=op0, op1=op1),
                        reads, writes, cost=ecost(eng, out))

        def stt(eng, out, in0, scalar, in1, op0, op1, reads, writes):
            emit_op(eng, lambda e: e.scalar_tensor_tensor(out=out, in0=in0, scalar=scalar, in1=in1, op0=op0, op1=op1),
                    reads, writes, cost=ecost(eng, out))

        def cp(eng, out, in_, reads, writes):
            if eng == "act":
                act(out, in_, AF.Copy, reads, writes)
            else:
                emit_op(eng, lambda e: e.tensor_copy(out=out, in_=in_), reads, writes, cost=ecost(eng, out))

        def memset(eng, ap, val, writes):
            emit_op(eng, lambda e: e.memset(ap, val), (), writes, cost=ecost(eng, ap))

        def dma(eng, out, in_, reads, writes, key):
            nbytes = 128 * fsz(out) * (4 if out.dtype == F32 else 2)
            emit_op(eng, lambda e: e.dma_start(out=out, in_=in_), reads, writes, dma=key, cost=2.0 + nbytes / 3.0e5)

        def tap(idx, src, reads):
            if DEBUG is None:
                return
            n = 1
            for d_ in src.shape[1:]:
                n *= d_
            dst = dbg_d[idx][:, 0:n]
            if len(src.shape) == 3:
                dst = dst.rearrange("p (a b) -> p a b", b=src.shape[2])
            dma("pool", dst, src, reads, [("dbg", idx)], ("dbg", idx))

        def redmax(out, in_, reads, writes):
            emit_op("dve", lambda e: e.tensor_reduce(out=out, in_=in_, axis=AX.X, op=ALU.max), reads, writes, cost=0.1)

        def recip(out, in_, reads, writes):
            emit_op("dve", lambda e: e.reciprocal(out=out, in_=in_), reads, writes, cost=0.1)

        def b3(ap2d, mid):
            n = ap2d.shape[1]
            return ap2d.unsqueeze(2).to_broadcast([ap2d.shape[0], n, mid])

        def v3(ap2d, inner):
            return ap2d.rearrange("p (a b) -> p a b", b=inner)

        def body(rec, seq):
            psc["F"] = 0
            psc["M"] = 0
            held.clear()
            STREAM["cur"] = "F"
            dma("sp", cst[:], cst_d, (), ["cst"], "par0")
            dma("sp", vbc[:], vbc_d, (), ["vbc"], "par1")
            dma("sp", vpp[:], vpp_d, (), ["vpp"], "par2")
            dma("sp", y1[:, 0:288], wsm_d, (), [("y1", 0)], "par3")
            cp("dve", wsm_bf[:], y1[:, 0:288], [("y1", 0)], ["wsm_bf"])
            cp("dve", cbf[:, 0:128], cst[:, 0:128], ["cst"], ["cbf0"])
            cp("dve", cbf[:, 128:384], cst[:, 384:640], ["cst"], ["cbf1"])
            CBF = ["cbf0", "cbf1"]
            act(A_bc[:], vbc[:, 1040:1056], AF.Exp, ["vbc"], ["A_bc"])
            ts("dve", A_bc[:], A_bc[:], -1.0, None, ALU.mult, None, ["A_bc"], ["A_bc"])
            memset("pool", state[:], 0.0, ["state"])
            memset("pool", state_bf[:], 0.0, ["state_bf"])
            memset("pool", halo_x[:], 0.0, ["halo_x"])
            memset("pool", halo_s[:], 0.0, ["halo_s"])
            WDT = wsm_bf[:].rearrange("p (k j) -> p k j", j=36)

            MODE = {"rec": rec}
            first_seen = set()
            RINGS = {"F": (0, RING_F), "M": (RING_F, RING_M)}
            ws = {k: {"next_load": 0, "next_use": 0, "released": set()} for k in RINGS}
            PIECE_COLS = {}
            for _p in range(NPIECE):
                PIECE_COLS[_p] = 3072 if 6 <= _p < 14 else 4096

            def slot_of(st, si):
                base, n = RINGS[st]
                return base + si % n

            def emit_load(st, si):
                piece = seq[st][si]
                slot = slot_of(st, si)
                ncol = PIECE_COLS[piece]
                nb = ncol // 2048 if ncol % 2048 == 0 else ncol // 1024
                bsz = ncol // nb
                if piece not in first_seen:
                    first_seen.add(piece)
                    dma("pool", ring[slot][:, 0:ncol].rearrange("p (a b) -> p a b", b=bsz),
                        wall_d[piece][:, 0:ncol].rearrange("p (a b) -> p a b", b=bsz), (), [("ring", slot)],
                        ("wc", slot))
                    dma("sp", scr_d[piece][:, 0:ncol], ring[slot][:, 0:ncol], [("ring", slot)], [("scr", piece)],
                        ("ws", slot))
                else:
                    dma("sp", ring[slot][:, 0:ncol], scr_d[piece][:, 0:ncol], [("scr", piece)], [("ring", slot)],
                        ("wl", slot))

            def pump(st):
                w = ws[st]
                n_slots = RINGS[st][1]
                while w["next_load"] < len(seq[st]):
                    n = w["next_load"]
                    if n >= n_slots and (n - n_slots) not in w["released"]:
                        break
                    emit_load(st, n)
                    w["next_load"] += 1

            def acquire(expect, st):
                if MODE["rec"]:
                    seq[st].append(expect)
                    si = len(seq[st]) - 1
                    sl = slot_of(st, si)
                    return ring[sl], ("ring", sl), (st, si)
                w = ws[st]
                si = w["next_use"]
                assert seq[st][si] == expect, (st, si, seq[st][si], expect)
                pump(st)
                assert w["next_load"] > si, ("weight ring over-subscribed", st, si, expect)
                w["next_use"] += 1
                sl = slot_of(st, si)
                return ring[sl], ("ring", sl), (st, si)

            def release(h):
                if MODE["rec"]:
                    return
                st, si = h
                ws[st]["released"].add(si)
                pump(st)

            def front_norm(blk, nt):
                for t in range(nt):
                    xs = xst[0]
                    xk_ = ("xst", 0)
                    if blk == 0:
                        memset("pool", xs[:], 0.0, [xk_])
                        dma("sp", xs[112:128, :], meta_d, (), [xk_], ("xl", 0))
                    else:
                        r0 = (blk - 1) * 512 + t * 128
                        dma("sp", xs[:], x_d[r0:r0 + 128, :], (), [xk_], ("xl", 0))
                    act(ub[:], xs[:], AF.Square, [xk_], ["ub", "ss"], accum=sm[:, 200:201])
                    act(sm[:, 204:205], sm[:, 200:201], AF.Ln, ["ss"], ["lnss"], bias=EPS, scale=1.0 / D)
                    act(sm[:, 208:209], sm[:, 204:205], AF.Exp, ["lnss"], ["rss"], scale=-0.5)
                    ts("dve", dgF[:], I_f, sm[:, 208:209], None, ALU.mult, None, ["cst", "rss"], ["dgF"])
                    for cg in range(2):
                        bi = newbank()
                        for c4 in range(4):
                            c = cg * 4 + c4
                            mm(pbank[bi][:, c4 * 128:(c4 + 1) * 128], xs[:, c * 128:(c + 1) * 128], dgF[:], True, True,
                               [xk_, "dgF"], [("ps", bi)])
                        tt("dve", uT[:, cg * 4:cg * 4 + 4, t * 128:(t + 1) * 128],
                           v3(pbank[bi][:, 0:512], 128), b3(GMIX[:, cg * 4:cg * 4 + 4], 128), ALU.mult,
                           [("ps", bi), "vpp"], [("uT", t, cg)])
                        relb(bi)
                    yield

            def back_load_x(blk):
                for t in range(4):
                    r0 = (blk - 1) * 512 + t * 128
                    dma("sp", hres[:, t, :], x_d[r0:r0 + 128, :], (), [("hres", t)], ("xh", t))

            def back_norm(nt):
                for t in range(nt):
                    act(ub2[:], hres[:, t, :], AF.Square, [("hres", t)], ["ub2", ("ss2", t)], accum=sm2[:, t:t + 1])
                act(sm2[:, 4:4 + nt], sm2[:, 0:nt], AF.Ln, [("ss2", t) for t in range(nt)], ["lnss2"],
                    bias=EPS, scale=1.0 / D)
                act(sm2[:, 8:8 + nt], sm2[:, 4:4 + nt], AF.Exp, ["lnss2"], ["rss2"], scale=-0.5)
                for t in range(nt):
                    ts("dve", dgM[:], I_f, sm2[:, 8 + t:9 + t], None, ALU.mult, None, ["cst", "rss2"], ["dgM"])
                    for cg in range(2):
                        bi = newbank()
                        for c4 in range(4):
                            c = cg * 4 + c4
                            mm(pbank[bi][:, c4 * 128:(c4 + 1) * 128], hres[:, t, c * 128:(c + 1) * 128], dgM[:], True, True,
                               [("hres", t), "dgM"], [("ps", bi)])
                        tt("dve", u2T[:, cg * 4:cg * 4 + 4, t * 128:(t + 1) * 128],
                           v3(pbank[bi][:, 0:512], 128), b3(GFFN[:, cg * 4:cg * 4 + 4], 128), ALU.mult,
                           [("ps", bi), "vpp"], [("u2T", t, cg)])
                        relb(bi)

            def u2T_keys(nt):
                return [("u2T", t, cg) for t in range(nt) for cg in range(2)]

            def uT_keys(nt):
                return [("uT", t, cg) for t in range(nt) for cg in range(2)]

            def inproj_z(nt):
                for half in range(2):
                    slot, skey, si = acquire(half, "F")
                    wv = slot[:].rearrange("p (k c) -> p k c", c=512)
                    for t in range(nt):
                        bi = newbank()
                        for k in range(8):
                            mm(pbank[bi][:, 0:512], uT[:, k, t * 128:(t + 1) * 128], wv[:, k, :], k == 0, k == 7,
                               [("uT", t, 0), ("uT", t, 1), skey], [("ps", bi)])
                        act(sz[:, t, half * 512:(half + 1) * 512], pbank[bi][:, 0:512], AF.Silu, [("ps", bi)],
                            [("sz", t, half)])
                        relb(bi)
                        yield
                    release(si)

            def inproj_dt(nt):
                for t in range(nt):
                    bi = newbank()
                    for k in range(8):
                        mm(pbank[bi][:, 0:16], uT[:, k, t * 128:(t + 1) * 128], WDT[:, k, 0:16], k == 0, k == 7,
                           [("uT", t, 0), ("uT", t, 1), "wsm_bf"], [("ps", bi)])
                    cp("dve", dtraw[:, t, :], pbank[bi][:, 0:16], [("ps", bi)], [("dtraw", t)])
                    relb(bi)

            def inproj_xbc(nt):
                W = nt * 128
                ukeys = uT_keys(nt)
                for i in range(4):
                    slot, skey, si = acquire(2 + i, "F")
                    wv = slot[:].rearrange("p (k c) -> p k c", c=512)
                    for cc in range(4):
                        ch = 4 * i + cc
                        bi = newbank()
                        for k in range(8):
                            mm(pbank[bi][:, 0:W], wv[:, k, cc * 128:(cc + 1) * 128], uT[:, k, 0:W], k == 0, k == 7,
                               ukeys + [skey], [("ps", bi)])
                        rb = raw[0]
                        rk = ("raw", 0)
                        ak = ("cacc", 0)
                        ab = cacc[0]
                        cp("pool", rb[:, 0:3], halo_x[:, ch, :], ["halo_x"], [rk])
                        cp("act", rb[:, 3:3 + W], pbank[bi][:, 0:W], [("ps", bi)], [rk])
                        relb(bi)
                        ts("pool", ab[:, 0:W], rb[:, 0:W], CW[:, ch * 4:ch * 4 + 1], None, ALU.mult, None,
                           [rk, "vpp"], [ak])
                        stt("dve", ab[:, 0:W], rb[:, 1:W + 1], CW[:, ch * 4 + 1:ch * 4 + 2], ab[:, 0:W], ALU.mult, ALU.add,
                            [rk, ak, "vpp"], [ak])
                        stt("dve", ab[:, 0:W], rb[:, 2:W + 2], CW[:, ch * 4 + 2:ch * 4 + 3], ab[:, 0:W], ALU.mult, ALU.add,
                            [rk, ak, "vpp"], [ak])
                        stt("dve", ab[:, 0:W], rb[:, 3:W + 3], CW[:, ch * 4 + 3:ch * 4 + 4], ab[:, 0:W], ALU.mult, ALU.add,
                            [rk, ak, "vpp"], [ak])
                        cp("pool", halo_x[:, ch, :], rb[:, W:W + 3], [rk], ["halo_x"])
                        act(xbcT[:, ch, 0:W], ab[:, 0:W], AF.Silu, [ak, "vpp"], [("xbcT", ch)], bias=CB[:, ch:ch + 1])
                        yield
                    release(si)

            def inproj_sc(nt, full):
                W = nt * 128
                ukeys = uT_keys(nt)
                names = ["C", "h"] + (["B"] if full else [])

                def tail(ch):
                    gv = xD[:, (ch % 2) * 512:(ch % 2) * 512 + 512]
                    gk = ("xD", ch % 2)
                    sq = yn[:, (ch % 2) * 512:(ch % 2) * 512 + 512]
                    qk = ("yn", ch % 2)
                    bi = newbank()
                    mm(pbank[bi][:, 0:W], BLK, sq[:, 0:W], True, True, [qk] + CBF, [("ps", bi)])
                    act(pbank[bi][:, 0:W], pbank[bi][:, 0:W], AF.Ln, [("ps", bi)], [("ps", bi)], bias=EPS, scale=1.0 / 64)
                    act(pbank[bi][:, 0:W], pbank[bi][:, 0:W], AF.Exp, [("ps", bi)], [("ps", bi)], scale=-0.5)
                    stt("dve", ycatT[:, 8 + ch, 0:W], gv[:, 0:W], GSC[:, ch:ch + 1], pbank[bi][:, 0:W], ALU.mult, ALU.mult,
                        [gk, ("ps", bi), "vpp"], [("ycatT", 8 + ch)])
                    relb(bi)

                pend = None
                for ch in range(8):
                    slot, skey, si = acquire(6 + ch, "F")
                    wv = slot[:, 0:3072].rearrange("p (k c) -> p k c", c=384)
                    banks = {}
                    for nm in names:
                        j = {"B": 0, "C": 1, "h": 2}[nm]
                        bi = newbank()
                        banks[nm] = bi
                        for k in range(8):
                            mm(pbank[bi][:, 0:W], wv[:, k, j * 128:(j + 1) * 128], uT[:, k, 0:W], k == 0, k == 7,
                               ukeys + [skey], [("ps", bi)])
                    release(si)
                    cp("pool", rawsc[:, 0:2], halo_s[:, ch, :], ["halo_s"], ["rawsc"])
                    cp("act", csb[:, 0:W], pbank[banks["C"]][:, 0:W], [("ps", banks["C"])], ["csb"])
                    tt("dve", rawsc[:, 2:2 + W], csb[:, 0:W], pbank[banks["h"]][:, 0:W], ALU.mult,
                       ["csb", ("ps", banks["h"])], ["rawsc"])
                    relb(banks["C"], banks["h"])
                    if full:
                        cp("act", bsb[:, 0:W], pbank[banks["B"]][:, 0:W], [("ps", banks["B"])], ["bsb"])
                        relb(banks["B"])
                        ts("pool", vsc[:, 0:W], rawsc[:, 0:W], SCW[:, ch * 3:ch * 3 + 1], None, ALU.mult, None,
                           ["rawsc", "vpp"], ["vsc"])
                        stt("dve", vsc[:, 0:W], rawsc[:, 1:W + 1], SCW[:, ch * 3 + 1:ch * 3 + 2], vsc[:, 0:W],
                            ALU.mult, ALU.add, ["rawsc", "vsc", "vpp"], ["vsc"])
                        stt("dve", vsc[:, 0:W], rawsc[:, 2:W + 2], SCW[:, ch * 3 + 2:ch * 3 + 3], vsc[:, 0:W],
                            ALU.mult, ALU.add, ["rawsc", "vsc", "vpp"], ["vsc"])
                    cp("pool", halo_s[:, ch, :], rawsc[:, W:W + 2], ["rawsc"], ["halo_s"])
                    if full:
                        gv = xD[:, (ch % 2) * 512:(ch % 2) * 512 + 512]
                        gk = ("xD", ch % 2)
                        sq = yn[:, (ch % 2) * 512:(ch % 2) * 512 + 512]
                        qk = ("yn", ch % 2)
                        tt("pool", gv[:, 0:W], vsc[:, 0:W], bsb[:, 0:W], ALU.mult, ["vsc", "bsb"], [gk])
                        act(sq[:, 0:W], gv[:, 0:W], AF.Square, [gk], [qk])
                        if pend is not None:
                            tail(pend)
                        pend = ch
                    yield
                if pend is not None:
                    tail(pend)

            def ssd_chunk(blk, t, need_y, prev_tail=None, holder=None):
                tsl = slice(t * 128, (t + 1) * 128)
                par = t % 2
                xk, bk = ("xtok", par), ("btok", par)
                for cg in range(2):
                    bi = newbank()
                    for c4 in range(4):
                        c = cg * 4 + c4
                        mm(pbank[bi][:, c4 * 128:(c4 + 1) * 128], xbcT[:, c, tsl], I_b, True, True,
                           [("xbcT", c)] + CBF, [("ps", bi)])
                    cp("act" if cg == 0 else "dve", xtok[par][:, cg * 512:(cg + 1) * 512], pbank[bi][:, 0:512],
                       [("ps", bi)], [xk])
                    relb(bi)
                bi = newbank()
                for c4 in range(4):
                    mm(pbank[bi][:, c4 * 128:(c4 + 1) * 128], xbcT[:, 8 + c4, tsl], I_b, True, True,
                       [("xbcT", 8 + c4)] + CBF, [("ps", bi)])
                cp("act", btok[par][:, 0:512], pbank[bi][:, 0:512], [("ps", bi)], [bk])
                relb(bi)
                yield
                XB, M_, NA, E1, L1, DT, DA = (sm[:, 0:16], sm[:, 16:32], sm[:, 32:48], sm[:, 48:64], sm[:, 64:80],
                                              sm[:, 80:96], sm[:, 96:112])
                tt("dve", XB, dtraw[:, t, :], DTB, ALU.add, [("dtraw", t), "vbc"], ["xb"])
                ts("dve", M_, XB, 0.0, None, ALU.max, None, ["xb"], ["m_"])
                stt("dve", NA, M_, -2.0, XB, ALU.mult, ALU.add, ["xb", "m_"], ["na"])
                act(E1, NA, AF.Exp, ["na"], ["e1"])
                act(L1, E1, AF.Ln, ["e1"], ["l1"], bias=1.0)
                tt("dve", DT, M_, L1, ALU.add, ["m_", "l1"], ["dt"])
                if blk == 0:
                    ts("dve", DT, DT, PADM, None, ALU.mult, None, ["dt", "cst"], ["dt"])
                tt("dve", DA, DT, A_bc[:], ALU.mult, ["dt", "A_bc"], ["da"])
                cbk = newbank()
                mm(pbank[cbk][:, 0:16], TRI, DA, True, True, ["da", "cst"], [("ps", cbk)])
                mm(pbank[cbk][:, 16:32], ONES, DA, True, True, ["da", "cst"], [("ps", cbk)])
                mm(pbank[cbk][0:16, 128:256], DA, TRI, True, True, ["da", "cst"], [("ps", cbk)])
                ACUM, DOUT, CD, TMP, DS, DTDS = (sm[:, 112:128], sm[:, 128:144], sm[:, 144:160], sm[:, 160:176],
                                                 sm[:, 176:192], sm[:, 224:240])
                cp("dve", ACUM, pbank[cbk][:, 0:16], [("ps", cbk)], ["acum"])
                act(CD, pbank[cbk][:, 16:32], AF.Exp, [("ps", cbk)], ["cd"])
                tt("dve", TMP, pbank[cbk][:, 16:32], ACUM, ALU.subtract, [("ps", cbk), "acum"], ["tmp"])
                act(DS, TMP, AF.Exp, ["tmp"], ["ds"])
                tt("dve", DTDS, DT, DS, ALU.mult, ["dt", "ds"], ["dtds"])
                tt("pool", v3(xdd[:], 64), v3(xtok[par][:], 64), b3(DTDS, 64), ALU.mult, [xk, "dtds"], ["xdd"])
                if not need_y:
                    relb(cbk)
                if need_y:
                    act(DOUT, pbank[cbk][:, 0:16], AF.Exp, [("ps", cbk)], ["dout"])
                    cp("act", acT[:], pbank[cbk][0:16, 128:256], [("ps", cbk)], ["acT"])
                    ts("dve", nacT[:], pbank[cbk][0:16, 128:256], -1.0, None, ALU.mult, None, [("ps", cbk)], ["nacT"])
                    tt("dve", v3(xdt[:], 64), v3(xtok[par][:], 64), b3(DT, 64), ALU.mult, [xk, "dt"], ["xdt"])
                    relb(cbk)
                    yield
                    cbb = newbank()
                    for g in range(4):
                        mm(pbank[cbb][:, g * 128:(g + 1) * 128], xbcT[:, 8 + g, tsl], xbcT[:, 12 + g, tsl], True, True,
                           [("xbcT", 8 + g), ("xbcT", 12 + g)], [("ps", cbb)])
                    ydb = [newbank(), newbank()]
                    for g in range(4):
                        sgb = newbank()
                        for r in range(4):
                            h = 4 * g + r
                            o = pbank[sgb][:, r * 128:(r + 1) * 128]
                            mm(o, selv(h), acT[:], True, False, ["cst", "acT"], [("ps", sgb)])
                            mm(o, nacT[:], selv(h), False, False, ["cst", "nacT"], [("ps", sgb)])
                            mm(o, I_b, MASKN, False, True, CBF, [("ps", sgb)])
                        lt = LTb[g % 2]
                        ltk = ("LT", g % 2)
                        act(lt[:], pbank[sgb][:, 0:512], AF.Exp, [("ps", sgb)], [ltk])
                        relb(sgb)
                        mt = MTb[g]
                        mtk = ("MT", g)
                        tt("dve", v3(mt[:], 128), v3(lt[:], 128),
                           pbank[cbb][:, g * 128:(g + 1) * 128].unsqueeze(1).to_broadcast([128, 4, 128]), ALU.mult,
                           [ltk, ("ps", cbb)], [mtk])
                        for r in range(4):
                            h = 4 * g + r
                            mm(pbank[ydb[h // 8]][:, (h % 8) * 64:(h % 8) * 64 + 64], mt[:, r * 128:(r + 1) * 128],
                               xdt[:, h * 64:(h + 1) * 64], True, True, [mtk, "xdt"], [("ps", ydb[h // 8])])
                        yield
                    relb(cbb)
                    yob = [newbank(), newbank()]
                    for g in range(4):
                        mm(pbank[yob[g // 2]][:, (g % 2) * 256:(g % 2) * 256 + 256], xbcT[:, 12 + g, tsl],
                           state_bf[:, g * 256:(g + 1) * 256], True, True, [("xbcT", 12 + g), "state_bf"],
                           [("ps", yob[g // 2])])
                    for hf in range(2):
                        tt("dve", v3(y1[:, hf * 512:(hf + 1) * 512], 64), v3(pbank[yob[hf]][:, 0:512], 64),
                           b3(DOUT[:, hf * 8:(hf + 1) * 8], 64), ALU.mult, [("ps", yob[hf]), "dout"], [("y1", hf)])
                        tt("dve", y1[:, hf * 512:(hf + 1) * 512], y1[:, hf * 512:(hf + 1) * 512], pbank[ydb[hf]][:, 0:512],
                           ALU.add, [("y1", hf), ("ps", ydb[hf])], [("y1", hf)])
                    relb(*yob)
                    relb(*ydb)
                stb = [newbank(), newbank()]
                for g in range(4):
                    mm(pbank[stb[g // 2]][:, (g % 2) * 256:(g % 2) * 256 + 256], btok[par][:, g * 128:(g + 1) * 128],
                       xdd[:, g * 256:(g + 1) * 256], True, True, [bk, "xdd"], [("ps", stb[g // 2])])
                tt("dve", v3(state[:], 64), v3(state[:], 64), b3(CD, 64), ALU.mult, ["state", "cd"], ["state"])
                for hf in range(2):
                    tt("dve", state[:, hf * 512:(hf + 1) * 512], state[:, hf * 512:(hf + 1) * 512], pbank[stb[hf]][:, 0:512],
                       ALU.add, ["state", ("ps", stb[hf])], ["state"])
                relb(*stb)
                cp("pool", state_bf[:], state[:], ["state"], ["state_bf"])
                yield
                if prev_tail is not None:
                    prev_tail()
                if not need_y:
                    return
                tt("pool", v3(xD[:], 64), v3(xtok[par][:], 64), b3(DSK, 64), ALU.mult, [xk, "vbc"], [("xD", 0), ("xD", 1)])
                tt("pool", y1[:], y1[:], xD[:], ALU.add, [("y1", 0), ("y1", 1), ("xD", 0), ("xD", 1)], [("y1", 0), ("y1", 1)])
                tt("pool", y1[:], y1[:], sz[:, t, :], ALU.mult, [("y1", 0), ("y1", 1), ("sz", t, 0), ("sz", t, 1)], [("y1", 0), ("y1", 1)])
                for g in range(4):
                    act(xD[:, g * 256:(g + 1) * 256], y1[:, g * 256:(g + 1) * 256], AF.Square, [("y1", 0), ("y1", 1), ("xD", g // 2)],
                        [("xD", g // 2), ("ssy", g)], accum=sm[:, 240 + g:241 + g])
                act(sm[:, 244:248], sm[:, 240:244], AF.Ln, [("ssy", g) for g in range(4)], ["lny"], bias=EPS, scale=1.0 / 256)
                act(sm[:, 248:252], sm[:, 244:248], AF.Exp, ["lny"], ["rsy"], scale=-0.5)
                tt("dve", v3(yn[:], 256), v3(y1[:], 256), b3(sm[:, 248:252], 256), ALU.mult, [("y1", 0), ("y1", 1), "rsy"], [("yn", 0), ("yn", 1)])

                def tail():
                    for cg in range(2):
                        bi = newbank()
                        for c4 in range(4):
                            c = cg * 4 + c4
                            mm(pbank[bi][:, c4 * 128:(c4 + 1) * 128], yn[:, c * 128:(c + 1) * 128], I_b, True, True,
                               [("yn", cg)] + CBF, [("ps", bi)])
                        tt("dve", ycatT[:, cg * 4:cg * 4 + 4, tsl], v3(pbank[bi][:, 0:512], 128),
                           b3(GSSD[:, cg * 4:cg * 4 + 4], 128), ALU.mult, [("ps", bi), "vpp"], [("ycatT_s", t, cg)])
                        relb(bi)

                holder["tail"] = tail

            def outproj(nt):
                slots = [acquire(14 + i, "M") for i in range(4)]
                for t in range(nt):
                    for half in range(2):
                        bi = newbank()
                        for kc in range(16):
                            slot, skey, si = slots[kc // 4]
                            wv = slot[:].rearrange("p (k c) -> p k c", c=1024)
                            rk = [("ycatT_s", t, kc // 4)] if kc < 8 else [("ycatT", kc)]
                            mm(pbank[bi][:, 0:512], ycatT[:, kc, t * 128:(t + 1) * 128],
                               wv[:, kc % 4, half * 512:(half + 1) * 512], kc == 0, kc == 15, rk + [skey], [("ps", bi)])
                        tt("dve", hres[:, t, half * 512:(half + 1) * 512], hres[:, t, half * 512:(half + 1) * 512],
                           pbank[bi][:, 0:512], ALU.add, [("hres", t), ("ps", bi)], [("hres", t)])
                        relb(bi)
                for s_ in slots:
                    release(s_[2])

            def router_tile(t):
                rt = rt_all[:, t * 160:(t + 1) * 160]
                if True:
                    bi = newbank()
                    for k in range(8):
                        mm(pbank[bi][:, 0:20], u2T[:, k, t * 128:(t + 1) * 128], WDT[:, k, 16:36], k == 0, k == 7,
                           [("u2T", t, 0), ("u2T", t, 1), "wsm_bf"], [("ps", bi)])
                    LG = rt[:, 0:20]
                    GL = rt[:, 0:4]
                    EL = rt[:, 4:20]
                    tt("dve", LG, pbank[bi][:, 0:20], RBIAS, ALU.add, [("ps", bi), "vbc"], [("lg", t)])
                    yield
                    relb(bi)
                    GMAX, NGMAX, GSUM, GVAL = rt[:, 20:21], rt[:, 21:22], rt[:, 22:23], rt[:, 23:24]
                    GM, GE, PEN = rt[:, 24:28], rt[:, 28:32], rt[:, 32:36]
                    redmax(GMAX, GL, [("lg", t)], [("gmax", t)])
                    yield
                    ts("dve", GM, GL, GMAX, None, ALU.is_equal, None, [("lg", t), ("gmax", t)], [("gm", t)])
                    yield
                    ts("dve", NGMAX, GMAX, -1.0, None, ALU.mult, None, [("gmax", t)], [("ngmax", t)])
                    yield
                    act(GE, GL, AF.Exp, [("lg", t), ("ngmax", t)], [("ge", t), ("gsum", t)], bias=NGMAX, scale=1.0, accum=GSUM)
                    yield
                    recip(GVAL, GSUM, [("gsum", t)], [("gval", t)])
                    yield
                    ts("dve", PEN, GM, BIG, -BIG, ALU.mult, ALU.add, [("gm", t)], [("pen", t)])
                    yield
                    ML, MK1, ML2, MK2 = rt[:, 36:52], rt[:, 52:68], rt[:, 68:84], rt[:, 84:100]
                    tt("dve", v3(ML, 4), v3(EL, 4), b3(PEN, 4), ALU.add, [("lg", t), ("pen", t)], [("ml", t)])
                    yield
                    M1, M2, DD, ED, W1, W2 = rt[:, 100:101], rt[:, 101:102], rt[:, 102:103], rt[:, 103:104], \
                        rt[:, 104:105], rt[:, 105:106]
                    redmax(M1, ML, [("ml", t)], [("m1", t)])
                    yield
                    ts("dve", MK1, ML, M1, None, ALU.is_equal, None, [("ml", t), ("m1", t)], [("mk1", t)])
                    yield
                    stt("dve", ML2, MK1, -BIG, ML, ALU.mult, ALU.add, [("mk1", t), ("ml", t)], [("ml2", t)])
                    yield
                    redmax(M2, ML2, [("ml2", t)], [("m2", t)])
                    yield
                    ts("dve", MK2, ML2, M2, None, ALU.is_equal, None, [("ml2", t), ("m2", t)], [("mk2", t)])
                    yield
                    tt("dve", DD, M2, M1, ALU.subtract, [("m1", t), ("m2", t)], [("dd", t)])
                    yield
                    act(ED, DD, AF.Exp, [("dd", t)], [("ed", t)])
                    yield
                    ts("dve", ED, ED, 1.0, None, ALU.add, None, [("ed", t)], [("ed", t)])
                    yield
                    recip(W1, ED, [("ed", t)], [("w1", t)])
                    yield
                    ts("dve", W2, W1, -1.0, 1.0, ALU.mult, ALU.add, [("w1", t)], [("w2", t)])
                    yield
                    C1, C2, COMB = rt[:, 112:128], rt[:, 128:144], rt[:, 144:160]
                    ts("dve", C1, MK1, W1, None, ALU.mult, None, [("mk1", t), ("w1", t)], [("c1", t)])
                    yield
                    stt("dve", C2, MK2, W2, C1, ALU.mult, ALU.add, [("mk2", t), ("w2", t), ("c1", t)], [("c2", t)])
                    yield
                    ts("dve", COMB, C2, GVAL, None, ALU.mult, None, [("c2", t), ("gval", t)], [("comb", t)])
                    yield
                    bj = newbank()
                    mm(pbank[bj][0:16, 0:128], COMB, I_f, True, True, [("comb", t), "cst"], [("ps", bj)])
                    cp("act", combT[:, t * 128:(t + 1) * 128], pbank[bj][0:16, 0:128], [("ps", bj)], [("combT", t)])
                    yield
                    relb(bj)

            def router(nt):
                gens = [router_tile(t) for t in range(nt)]
                alive = [True] * nt
                while any(alive):
                    for t in range(nt):
                        if alive[t]:
                            try:
                                next(gens[t])
                            except StopIteration:
                                alive[t] = False

            def moe(nt):
                W = nt * 128
                ukeys = u2T_keys(nt)
                ctk = [("combT", t) for t in range(nt)]

                def down(e, hb, hk):
                    sd = acquire(20 + 3 * e, "M")
                    wd = sd[0][:].rearrange("p (k c) -> p k c", c=1024)
                    for t in range(nt):
                        for half in range(2):
                            bd = newbank()
                            for j in range(4):
                                mm(pbank[bd][:, 0:512], hb[:, j, t * 128:(t + 1) * 128],
                                   wd[:, j, half * 512:(half + 1) * 512], j == 0, j == 3, [(hk, j), sd[1]], [("ps", bd)])
                            tt("dve", hres[:, t, half * 512:(half + 1) * 512], hres[:, t, half * 512:(half + 1) * 512],
                               pbank[bd][:, 0:512], ALU.add, [("hres", t), ("ps", bd)], [("hres", t)])
                            relb(bd)
                        yield
                    release(sd[2])

                pend = None
                for e in range(16):
                    sg = acquire(18 + 3 * e, "M")
                    su = acquire(19 + 3 * e, "M")
                    wg = sg[0][:].rearrange("p (k c) -> p k c", c=512)
                    wu = su[0][:].rearrange("p (k c) -> p k c", c=512)
                    bi = newbank()
                    mm(pbank[bi][:, 0:W], selv(e), combT[:, 0:W], True, True, ["cst"] + ctk, [("ps", bi)])
                    act(cb_sb[:, 0:W], pbank[bi][:, 0:W], AF.Copy, [("ps", bi)], ["cb_sb"], scale=0.5)
                    relb(bi)
                    hb = hT[e % 2]
                    hk = ("hT", e % 2)
                    for j in range(4):
                        bg = newbank()
                        for k in range(8):
                            mm(pbank[bg][:, 0:W], wg[:, k, j * 128:(j + 1) * 128], u2T[:, k, 0:W], k == 0, k == 7,
                               ukeys + [sg[1]], [("ps", bg)])
                        bu = newbank()
                        for k in range(8):
                            mm(pbank[bu][:, 0:W], wu[:, k, j * 128:(j + 1) * 128], u2T[:, k, 0:W], k == 0, k == 7,
                               ukeys + [su[1]], [("ps", bu)])
                        sbuf_s = s_sb[0]
                        sk = ("s_sb", 0)
                        tb = t_sb[j % 2]
                        tk = ("t_sb", j % 2)
                        act(sbuf_s[:, 0:W], pbank[bg][:, 0:W], AF.Tanh, [("ps", bg)], [sk], scale=0.5)
                        stt("dve", tb[:, 0:W], sbuf_s[:, 0:W], 1.0, pbank[bg][:, 0:W], ALU.add, ALU.mult,
                            [sk, ("ps", bg)], [tk])
                        tt("dve", tb[:, 0:W], tb[:, 0:W], pbank[bu][:, 0:W], ALU.mult, [tk, ("ps", bu)], [tk])
                        relb(bg, bu)
                        tt("pool", hb[:, j, 0:W], tb[:, 0:W], cb_sb[:, 0:W], ALU.mult, [tk, "cb_sb"], [(hk, j)])
                        yield
                    release(sg[2])
                    release(su[2])
                    if pend is not None:
                        yield from down(*pend)
                    pend = (e, hb, hk)
                yield from down(*pend)

            def final_norm_store(blk, nt):
                for t in range(nt):
                    act(ub2[:], hres[:, t, :], AF.Square, [("hres", t)], ["ub2", ("ssf", t)], accum=sm2[:, 12 + t:13 + t])
                act(sm2[:, 16:16 + nt], sm2[:, 12:12 + nt], AF.Ln, [("ssf", t) for t in range(nt)], ["lnf"],
                    bias=EPS, scale=1.0 / D)
                act(sm2[:, 20:20 + nt], sm2[:, 16:16 + nt], AF.Exp, ["lnf"], ["rsf"], scale=-0.5)
                for t in range(nt):
                    stt("dve", hres[:, t, :], hres[:, t, :], sm2[:, 20 + t:21 + t], GFIN, ALU.mult,
                        ALU.mult, [("hres", t), "rsf", "vbc"], [("hres", t)])
                    r0 = (blk - 1) * 512 + t * 128
                    dma("sp", out_d[r0:r0 + 128, :], hres[:, t, :], [("hres", t)], [("out", r0)], ("os", t))


            def drain(g):
                for _ in g:
                    pass

            def front(blk, nt, full):
                yield from front_norm(blk, nt)
                if full:
                    yield from inproj_z(nt)
                inproj_dt(nt)
                yield from inproj_xbc(nt)
                marker("NEED", "outproj_done")
                yield from inproj_sc(nt, full)
                tail_fn = None
                for t in range(nt):
                    holder = {}
                    yield from ssd_chunk(blk, t, full, tail_fn, holder)
                    tail_fn = holder.get("tail")
                if tail_fn is not None:
                    tail_fn()

            def interleave(gm, gf, ratio):
                alive_m, alive_f = True, gf is not None
                while alive_m or alive_f:
                    for _ in range(ratio):
                        if alive_m:
                            try:
                                next(gm)
                            except StopIteration:
                                alive_m = False
                    if alive_f:
                        try:
                            next(gf)
                        except StopIteration:
                            alive_f = False

            def record(stream, fn):
                REC["list"] = []
                STREAM["cur"] = stream
                fn()
                lst = REC["list"]
                REC["list"] = None
                STREAM["cur"] = "F"
                return lst

            def merge(lM, lF):
                out = []
                idx = {"M": 0, "F": 0}
                lists = {"M": lM, "F": lF}
                n = {"M": max(1, len(lM)), "F": max(1, len(lF))}
                marks = set()
                eng_free = {e: 0.0 for e in ENGS}
                wr = {}
                rd = {}

                def avail(st):
                    i = idx[st]
                    if i >= len(lists[st]):
                        return False
                    it = lists[st][i]
                    if it[0] == "NEED" and it[1] not in marks:
                        return False
                    return True

                def est(item):
                    if item[0] in ("MARK", "NEED"):
                        return -1.0
                    eng = item[0]
                    t = eng_free[eng]
                    for k in item[2]:
                        w = wr.get(k)
                        if w is not None:
                            t = max(t, w[0] + (XLAT if w[1] != eng else 0.0))
                    for k in item[3]:
                        w = wr.get(k)
                        if w is not None:
                            t = max(t, w[0] + (XLAT if w[1] != eng else 0.0))
                        r = rd.get(k)
                        if r is not None:
                            t = max(t, r + XLAT)
                    return t

                while idx["M"] < len(lM) or idx["F"] < len(lF):
                    aM, aF = avail("M"), avail("F")
                    assert aM or aF, "unsatisfied cross-stream marker"
                    if aM and aF:
                        tM = est(lM[idx["M"]])
                        tF = est(lF[idx["F"]])
                        if abs(tM - tF) < 1e-9:
                            pick = "F" if idx["F"] / n["F"] <= idx["M"] / n["M"] else "M"
                        else:
                            pick = "M" if tM < tF else "F"
                    else:
                        pick = "M" if aM else "F"
                    item = lists[pick][idx[pick]]
                    idx[pick] += 1
                    if item[0] == "MARK":
                        marks.add(item[1])
                        continue
                    if item[0] == "NEED":
                        continue
                    start = est(item)
                    eng, cost = item[0], item[5]
                    if item[4] is not None:
                        eng_free[eng] = start + 0.06
                        end = start + cost
                    else:
                        end = start + cost
                        eng_free[eng] = end
                    for k in item[2]:
                        rd[k] = max(rd.get(k, 0.0), end)
                    for k in item[3]:
                        wr[k] = (end, eng)
                        rd[k] = 0.0
                    out.append(item[:5])
                return out

            def back(blk):
                back_load_x(blk)
                outproj(4)
                marker("MARK", "outproj_done")
                back_norm(4)
                router(4)
                drain(moe(4))
                final_norm_store(blk, 4)

            def schedule(ops, sid):
                n_ = len(ops)
                preds = [set() for _ in range(n_)]
                last_w = {}
                readers = {}
                for i, op in enumerate(ops):
                    for k in op[2]:
                        w = last_w.get(k)
                        if w is not None:
                            preds[i].add(w)
                    for k in op[3]:
                        w = last_w.get(k)
                        if w is not None:
                            preds[i].add(w)
                        for r in readers.get(k, ()):
                            preds[i].add(r)
                    for k in op[2]:
                        readers.setdefault(k, []).append(i)
                    for k in op[3]:
                        last_w[k] = i
                        readers[k] = []
                    preds[i].discard(i)
                succs = [[] for _ in range(n_)]
                indeg = [0] * n_
                for i in range(n_):
                    indeg[i] = len(preds[i])
                    for p in preds[i]:
                        succs[p].append(i)
                eng_free = {e: 0.0 for e in ENGS}
                fin = [0.0] * n_
                rdy = [0.0] * n_
                ready = set(i for i in range(n_) if indeg[i] == 0)
                order = []
                pos = [0] * n_
                cnt_s = {}
                for i in range(n_):
                    pos[i] = cnt_s.get(sid[i], 0)
                    cnt_s[sid[i]] = pos[i] + 1
                done_s = {k: [False] * v for k, v in cnt_s.items()}
                oldest = {k: 0 for k in cnt_s}
                while ready:
                    best = None
                    bt = None
                    for i in ready:
                        if pos[i] > oldest[sid[i]] + LOOKAHEAD:
                            continue
                        t = max(eng_free[ops[i][0]], rdy[i])
                        if bt is None or t < bt - 1e-9 or (abs(t - bt) <= 1e-9 and i < best):
                            best, bt = i, t
                    ready.discard(best)
                    st_ = sid[best]
                    done_s[st_][pos[best]] = True
                    while oldest[st_] < len(done_s[st_]) and done_s[st_][oldest[st_]]:
                        oldest[st_] += 1
                    op = ops[best]
                    eng, cost = op[0], op[5]
                    if op[4] is not None:
                        eng_free[eng] = bt + 0.06
                    else:
                        eng_free[eng] = bt + cost
                    fin[best] = bt + cost
                    order.append(best)
                    for s_ in succs[best]:
                        lat = XLAT if (ops[s_][0] != eng or op[4] is not None) else 0.0
                        if fin[best] + lat > rdy[s_]:
                            rdy[s_] = fin[best] + lat
                        indeg[s_] -= 1
                        if indeg[s_] == 0:
                            ready.add(s_)
                assert len(order) == n_
                return [ops[i][:5] for i in order]

            def window(lM, lF):
                cut = 0
                for i, it in enumerate(lM):
                    if it[0] == "MARK":
                        cut = i
                        break
                base = [(it, "M") for it in lM[:cut]] + [(it, "F") for it in lF] + [(it, "M") for it in lM[cut:]]
                base = [b_ for b_ in base if b_[0][0] not in ("MARK", "NEED")]
                return [b_[0] for b_ in base], [b_[1] for b_ in base]

            l0 = record("F", lambda: drain(front(0, 1, False)))
            l1 = record("F", lambda: drain(front(1, 4, True)))
            for item in schedule(*window([], l0 + l1)):
                SS[0].add(*item)
            for blk in range(1, 9):
                lM = record("M", lambda: back(blk))
                lF = record("F", lambda: drain(front(blk + 1, 4, True))) if blk < 8 else []
                for item in schedule(*window(lM, lF)):
                    SS[0].add(*item)
            SS[0].add("sp", None, reads=[("out", r) for r in range(0, SEQ, 128)])

        seq = {"F": [], "M": []}
        body(True, seq)
        print("sbuf bytes remaining", nc.sbuf_bytes_remaining)
        SS[0] = Sched()
        body(False, seq)
        SS[0].emit(nc)
    return nc


def _piece(w, rows_per_chunk_cols):
    K, C = w.shape
    nk = K // 128
    return np.ascontiguousarray(w.reshape(nk, 128, C).transpose(1, 0, 2).reshape(128, nk * C))


def _host_layout(inputs):
    f = lambda a: np.asarray(a, dtype=np.float32)
    w_in = f(inputs["w_in"])[0]
    w_out = f(inputs["w_out"])[0]
    wg = f(inputs["expert_w_gate"])[0]
    wu = f(inputs["expert_w_up"])[0]
    wd = f(inputs["expert_w_down"])[0]
    wall = np.zeros((NPIECE, 128, 4096), np.float32)
    wall[0] = _piece(w_in[:, 0:512], None)
    wall[1] = _piece(w_in[:, 512:1024], None)
    for i in range(4):
        wall[2 + i] = _piece(w_in[:, 1024 + 512 * i:1024 + 512 * (i + 1)], None)
    o = 3088
    for ch in range(8):
        cols = np.concatenate([w_in[:, o + 1024 * j + 128 * ch:o + 1024 * j + 128 * (ch + 1)] for j in range(3)], axis=1)
        wall[6 + ch][:, 0:3072] = _piece(cols, None)
    for i in range(4):
        wall[14 + i] = _piece(w_out[512 * i:512 * (i + 1), :], None)
    for e in range(16):
        wall[18 + 3 * e] = _piece(wg[e], None)
        wall[19 + 3 * e] = _piece(wu[e], None)
        wall[20 + 3 * e] = _piece(wd[e], None)
    wsmall_src = np.concatenate([w_in[:, 3072:3088], f(inputs["router_group_w"])[0], f(inputs["router_expert_w"])[0]],
                                axis=1)
    wsmall = _piece(wsmall_src, None)
    vrow = np.concatenate([f(inputs["norm_final"]), f(inputs["dt_bias"])[0], f(inputs["A_log"])[0],
                           f(inputs["D_skip"])[0], f(inputs["router_group_b"])[0], f(inputs["router_expert_b"])[0]])
    vbc = np.ascontiguousarray(np.broadcast_to(vrow[None, :], (128, vrow.shape[0])))
    pp = lambda v: np.ascontiguousarray(v.reshape(-1, 128).T)
    cw = f(inputs["ssd_conv_w"])[0]
    cwpp = np.ascontiguousarray(cw.reshape(4, 16, 128).transpose(2, 1, 0).reshape(128, 64))
    scw = f(inputs["sc_conv_w"])[0]
    scwpp = np.ascontiguousarray(scw.reshape(3, 8, 128).transpose(2, 1, 0).reshape(128, 24))
    vpp = np.concatenate([pp(f(inputs["norm_mix"])[0]), pp(f(inputs["norm_ffn"])[0]), pp(f(inputs["ssd_norm"])[0]),
                          pp(f(inputs["sc_norm"])[0]), cwpp, pp(f(inputs["ssd_conv_b"])[0]), scwpp], axis=1)
    vpp = np.ascontiguousarray(vpp.astype(np.float32))
    cst = np.zeros((128, 641), np.float32)
    cst[:, 0:128] = np.eye(128)
    cst[:, 128:256] = np.triu(np.ones((128, 128)))
    cst[:, 256:384] = 1.0
    cst[:, 384:512] = np.where(np.arange(128)[None, :] >= np.arange(128)[:, None], 0.0, -BIG)
    blk = np.zeros((128, 128), np.float32)
    blk[0:64, 0:64] = 1.0
    blk[64:128, 64:128] = 1.0
    cst[:, 512:640] = blk
    cst[112:128, 640] = 1.0
    shared = {"meta": f(inputs["meta_tokens"]), "wall": wall, "wsmall": wsmall, "vbc": vbc, "vpp": vpp, "cst": cst}
    return shared


_NC_CACHE = {}


def kernel(**inputs):
    x = np.asarray(inputs["x"], dtype=np.float32)
    shared = _host_layout(inputs)
    if "nc" not in _NC_CACHE:
        _NC_CACHE["nc"] = build_program()
    nc = _NC_CACHE["nc"]
    in_maps = []
    for c in range(NCORES):
        m = dict(shared)
        m["x"] = np.ascontiguousarray(x[c])
        in_maps.append(m)
    res = run_bass_kernel_spmd(nc, in_maps, core_ids=list(range(NCORES)))
    out = np.stack([np.asarray(r["out"], dtype=np.float32) for r in res.results], axis=0)
    if DEBUG is not None:
        kernel.dbg = [r.get("dbg") for r in res.results]
    return out
```

```python
from contextlib import ExitStack

import numpy as np
import concourse.bass as bass
import concourse.mybir as mybir
from concourse.bass_utils import run_bass_kernel_spmd

AF = mybir.ActivationFunctionType
ALU = mybir.AluOpType
AX = mybir.AxisListType
F32 = mybir.dt.float32
BF16 = mybir.dt.bfloat16

D = 1024
SEQ = 4096
NCORES = 8
EPS = 1e-6
NPIECE = 66
RING_F = 2
RING_M = 4
DGE_SCRATCH = 16384
RING = RING_F + RING_M
BIG = 30000.0
FSPEED = 1.1
HOTGAP = 0
XLAT = 3.0
LOOKAHEAD = 0
PECYC = 2000.0
MAXSKEW = 0.12
DEBUG = None

ENGS = ["pe", "act", "dve", "pool", "sp"]
SEM_LIMIT = 20000


class _Op:
    __slots__ = ("eng", "fn", "deps", "signal", "dma", "dma_val", "epoch", "cnt", "waits")

    def __init__(self, eng, fn, dma):
        self.eng = eng
        self.fn = fn
        self.dma = dma
        self.deps = []
        self.signal = False
        self.dma_val = 0
        self.epoch = 0
        self.cnt = 0
        self.waits = {}


class Sched:
    def __init__(self):
        self.q = {e: [] for e in ENGS}
        self.last_w = {}
        self.readers = {}
        self.dma_count = {}

    def add(self, eng, fn, reads=(), writes=(), dma=None):
        op = _Op(eng, fn, dma)
        deps = []
        for r in reads:
            w = self.last_w.get(r)
            if w is not None:
                deps.append(w)
        for b in writes:
            lw = self.last_w.get(b)
            if lw is not None and (lw.eng != eng or lw.dma or dma or eng != "pe"):
                deps.append(lw)
            for rd in self.readers.get(b, ()):
                if rd.eng != eng or rd.dma or dma:
                    deps.append(rd)
        op.deps = deps
        for r in reads:
            self.readers.setdefault(r, []).append(op)
        for b in writes:
            self.last_w[b] = op
            self.readers[b] = []
        if dma is not None:
            self.dma_count[dma] = self.dma_count.get(dma, 0) + 16
            op.dma_val = self.dma_count[dma]
        self.q[eng].append(op)
        return op

    def finalize(self):
        for e in ENGS:
            for op in self.q[e]:
                for d in op.deps:
                    if d.dma is None:
                        d.signal = True
        self.sem_keys = set()
        for e in ENGS:
            cnt = 0
            epoch = 0
            for op in self.q[e]:
                if op.dma is None and op.signal:
                    cnt += 1
                    if cnt > SEM_LIMIT:
                        epoch += 1
                        cnt = 1
                    op.epoch = epoch
                    op.cnt = cnt
                    self.sem_keys.add(("c", e, epoch))
                if op.dma is not None:
                    self.sem_keys.add(("d", op.dma))
        for e in ENGS:
            waited = {}
            for op in self.q[e]:
                w = {}
                for d in op.deps:
                    if d.dma is not None:
                        key, val = ("d", d.dma), d.dma_val
                    else:
                        key, val = ("c", d.eng, d.epoch), d.cnt
                    if waited.get(key, 0) >= val:
                        continue
                    if w.get(key, 0) < val:
                        w[key] = val
                for k, v in w.items():
                    waited[k] = v
                op.waits = w

    def emit(self, nc):
        self.finalize()
        with ExitStack() as es:
            sems = {}
            for i, k in enumerate(sorted(self.sem_keys, key=str)):
                sems[k] = es.enter_context(nc.semaphore("s%d" % i))
            block = es.enter_context(nc.Block())

            def run(e, eng):
                for op in self.q[e]:
                    for k, v in op.waits.items():
                        eng.wait_ge(sems[k], v)
                    if op.fn is None:
                        continue
                    inst = op.fn(eng)
                    if op.dma is not None:
                        inst.then_inc(sems[("d", op.dma)], 16)
                    elif op.signal:
                        inst.then_inc(sems[("c", e, op.epoch)], 1)

            @block.tensor
            def _(eng):
                run("pe", eng)

            @block.scalar
            def _(eng):
                run("act", eng)

            @block.vector
            def _(eng):
                run("dve", eng)

            @block.gpsimd
            def _(eng):
                run("pool", eng)

            @block.sync
            def _(eng):
                run("sp", eng)


def build_program():
    nc = bass.Bass("TRN2", target_bir_lowering=False, dynamic_dma_scratch_size=DGE_SCRATCH)
    SS = [Sched()]
    REC = {"list": None}
    STREAM = {"cur": "F"}

    def emit_op(eng, fn, reads=(), writes=(), dma=None, cost=0.3):
        if REC["list"] is not None:
            REC["list"].append((eng, fn, tuple(reads), tuple(writes), dma, cost))
        else:
            SS[0].add(eng, fn, reads, writes, dma)

    def marker(kind, name):
        if REC["list"] is not None:
            REC["list"].append((kind, name))

    def din(name, shape, dt=F32):
        return nc.dram_tensor(name, shape, dt, kind="ExternalInput").ap()

    x_d = din("x", [SEQ, D])
    meta_d = din("meta", [16, D])
    wall_d = din("wall", [NPIECE, 128, 4096])
    wsm_d = din("wsmall", [128, 8 * 36])
    vbc_d = din("vbc", [128, 1092])
    vpp_d = din("vpp", [128, 136])
    cst_d = din("cst", [128, 641])
    out_d = nc.dram_tensor("out", [SEQ, D], F32, kind="ExternalOutput").ap()
    scr_d = nc.dram_tensor("scr", [NPIECE, 128, 4096], BF16, kind="Internal").ap()
    dbg_d = None
    if DEBUG is not None:
        dbg_d = nc.dram_tensor("dbg", [8, 128, 4096], F32, kind="ExternalOutput").ap()

    es = ExitStack()
    with es:
        def sb(name, shape, dt=F32):
            return es.enter_context(nc.sbuf_tensor("sb_" + name, shape, dt))

        ring = [sb("ring%d" % i, [128, 4096], BF16) for i in range(RING)]
        hres = sb("hres", [128, 4, 1024])
        xst = [sb("xst%d" % i, [128, 1024]) for i in range(1)]
        uT = sb("uT", [128, 8, 512], BF16)
        u2T = sb("u2T", [128, 8, 512], BF16)
        ub = sb("ub", [128, 1024], BF16)
        ub2 = sb("ub2", [128, 1024], BF16)
        bsb = sb("bsb", [128, 512])
        dgF = sb("dgF", [128, 128])
        dgM = sb("dgM", [128, 128])
        sm2 = sb("sm2", [128, 32])
        sz = sb("sz", [128, 4, 1024], BF16)
        xbcT = sb("xbcT", [128, 16, 512], BF16)
        ycatT = sb("ycatT", [128, 16, 512], BF16)
        dtraw = sb("dtraw", [128, 4, 16])
        raw = [sb("raw%d" % i, [128, 515]) for i in range(1)]
        cacc = [sb("cacc%d" % i, [128, 512]) for i in range(1)]
        halo_x = sb("halo_x", [128, 16, 3])
        halo_s = sb("halo_s", [128, 8, 2])
        csb = sb("csb", [128, 512])
        rawsc = sb("rawsc", [128, 514])
        vsc = sb("vsc", [128, 512])
        xtok = [sb("xtok%d" % i, [128, 1024], BF16) for i in range(2)]
        btok = [sb("btok%d" % i, [128, 512], BF16) for i in range(2)]
        xdt = sb("xdt", [128, 1024], BF16)
        xdd = sb("xdd", [128, 1024], BF16)
        LTb = [sb("LT%d" % i, [128, 512], BF16) for i in range(2)]
        MTb = [sb("MT%d" % i, [128, 512], BF16) for i in range(4)]
        state = sb("state", [128, 1024])
        state_bf = sb("state_bf", [128, 1024], BF16)
        y1 = sb("y1", [128, 1024])
        xD = sb("xD", [128, 1024])
        yn = sb("yn", [128, 1024], BF16)
        sm = sb("sm", [128, 256])
        acT = sb("acT", [16, 128])
        nacT = sb("nacT", [16, 128])
        rt_all = sb("rt", [128, 640])
        combT = sb("combT", [16, 512])
        cb_sb = sb("cb_sb", [128, 512])
        s_sb = [sb("s_sb%d" % i, [128, 512]) for i in range(1)]
        t_sb = [sb("t_sb%d" % i, [128, 512]) for i in range(2)]
        hT = [sb("hT%d" % i, [128, 4, 512], BF16) for i in range(2)]
        cst = sb("cst", [128, 641])
        vbc = sb("vbc", [128, 1092])
        vpp = sb("vpp", [128, 136])
        wsm_bf = sb("wsm_bf", [128, 288], BF16)
        cbf = sb("cbf", [128, 384], BF16)
        A_bc = sb("A_bc", [128, 16])
        pbank = [es.enter_context(nc.psum_tensor("pb%d" % i, [128, 512], F32)) for i in range(8)]

        I_f = cst[:, 0:128]
        TRI = cst[:, 128:256]
        ONES = cst[:, 256:384]
        PADM = cst[:, 640:641]
        I_b = cbf[:, 0:128]
        MASKN = cbf[:, 128:256]
        BLK = cbf[:, 256:384]
        GFIN = vbc[:, 0:1024]
        DTB = vbc[:, 1024:1040]
        DSK = vbc[:, 1056:1072]
        RBIAS = vbc[:, 1072:1092]
        GMIX = vpp[:, 0:8]
        GFFN = vpp[:, 8:16]
        GSSD = vpp[:, 16:24]
        GSC = vpp[:, 24:32]
        CW = vpp[:, 32:96]
        CB = vpp[:, 96:112]
        SCW = vpp[:, 112:136]

        def selv(h):
            return cst[0:16, h:h + 1].to_broadcast([16, 128])


        BANKS = {"F": [0, 1, 2, 3], "M": [4, 5, 6, 7]}
        held = set()
        psc = {"F": 0, "M": 0}

        def newbank():
            st = STREAM["cur"]
            bl = BANKS[st]
            for _ in range(len(bl)):
                i = bl[psc[st] % len(bl)]
                psc[st] += 1
                if i not in held:
                    held.add(i)
                    return i
            raise RuntimeError("no free PSUM bank for stream " + st)

        def relb(*bs):
            for b_ in bs:
                held.discard(b_)

        def fsz(ap):
            n = 1
            for d_ in ap.shape[1:]:
                n *= d_
            return n

        def ecost(eng, out):
            n = fsz(out)
            if eng == "dve":
                return 0.07 + n / 960.0
            if eng == "pool":
                return 0.3 + n / 550.0
            return 0.22 + n / 1200.0

        def mm(out, lhsT, rhs, start, stop, reads, writes):
            passes = 4 if lhsT.dtype == F32 else 1
            c = max(fsz(out) * passes / PECYC, 0.055)
            emit_op("pe", lambda e: e.matmul(out, lhsT=lhsT, rhs=rhs, start=start, stop=stop), reads, writes, cost=c)

        def act(out, in_, func, reads, writes, bias=None, scale=None, accum=None):
            kw = {}
            if bias is not None:
                kw["bias"] = bias
            if scale is not None:
                kw["scale"] = scale
            if accum is not None:
                kw["accum_out"] = accum
            emit_op("act", lambda e: e.activation(out=out, in_=in_, func=func, **kw), reads, writes,
                    cost=0.22 + fsz(in_) / 1200.0)

        def tt(eng, out, in0, in1, op, reads, writes):
            emit_op(eng, lambda e: e.tensor_tensor(out=out, in0=in0, in1=in1, op=op), reads, writes, cost=ecost(eng, out))

        def ts(eng, out, in0, s1, s2, op0, op1, reads, writes):
            if s2 is None:
                emit_op(eng, lambda e: e.tensor_scalar(out=out, in0=in0, scalar1=s1, scalar2=None, op0=op0), reads, writes,
                        cost=ecost(eng, out))
            else:
                emit_op(eng, lambda e: e.tensor_scalar(out=out, in0=in0, scalar1=s1, scalar2=s2, op0=op0, op1=op1),
                        reads, writes, cost=ecost(eng, out))

        def stt(eng, out, in0, scalar, in1, op0, op1, reads, writes):
            emit_op(eng, lambda e: e.scalar_tensor_tensor(out=out, in0=in0, scalar=scalar, in1=in1, op0=op0, op1=op1),
                    reads, writes, cost=ecost(eng, out))

        def cp(eng, out, in_, reads, writes):
            if eng == "act":
                act(out, in_, AF.Copy, reads, writes)
            else:
                emit_op(eng, lambda e: e.tensor_copy(out=out, in_=in_), reads, writes, cost=ecost(eng, out))

        def memset(eng, ap, val, writes):
            emit_op(eng, lambda e: e.memset(ap, val), (), writes, cost=ecost(eng, ap))

        def dma(eng, out, in_, reads, writes, key):
            nbytes = 128 * fsz(out) * (4 if out.dtype == F32 else 2)
            emit_op(eng, lambda e: e.dma_start(out=out, in_=in_), reads, writes, dma=key, cost=2.0 + nbytes / 3.0e5)

        def tap(idx, src, reads):
            if DEBUG is None:
                return
            n = 1
            for d_ in src.shape[1:]:
                n *= d_
            dst = dbg_d[idx][:, 0:n]
            if len(src.shape) == 3:
                dst = dst.rearrange("p (a b) -> p a b", b=src.shape[2])
            dma("pool", dst, src, reads, [("dbg", idx)], ("dbg", idx))

        def redmax(out, in_, reads, writes):
            emit_op("dve", lambda e: e.tensor_reduce(out=out, in_=in_, axis=AX.X, op=ALU.max), reads, writes, cost=0.1)

        def recip(out, in_, reads, writes):
            emit_op("dve", lambda e: e.reciprocal(out=out, in_=in_), reads, writes, cost=0.1)

        def b3(ap2d, mid):
            n = ap2d.shape[1]
            return ap2d.unsqueeze(2).to_broadcast([ap2d.shape[0], n, mid])

        def v3(ap2d, inner):
            return ap2d.rearrange("p (a b) -> p a b", b=inner)

        def body(rec, seq):
            psc["F"] = 0
            psc["M"] = 0
            held.clear()
            STREAM["cur"] = "F"
            dma("sp", cst[:], cst_d, (), ["cst"], "par0")
            dma("sp", vbc[:], vbc_d, (), ["vbc"], "par1")
            dma("sp", vpp[:], vpp_d, (), ["vpp"], "par2")
            dma("sp", y1[:, 0:288], wsm_d, (), [("y1", 0)], "par3")
            cp("dve", wsm_bf[:], y1[:, 0:288], [("y1", 0)], ["wsm_bf"])
            cp("dve", cbf[:, 0:128], cst[:, 0:128], ["cst"], ["cbf0"])
            cp("dve", cbf[:, 128:384], cst[:, 384:640], ["cst"], ["cbf1"])
            CBF = ["cbf0", "cbf1"]
            act(A_bc[:], vbc[:, 1040:1056], AF.Exp, ["vbc"], ["A_bc"])
            ts("dve", A_bc[:], A_bc[:], -1.0, None, ALU.mult, None, ["A_bc"], ["A_bc"])
            memset("pool", state[:], 0.0, ["state"])
            memset("pool", state_bf[:], 0.0, ["state_bf"])
            memset("pool", halo_x[:], 0.0, ["halo_x"])
            memset("pool", halo_s[:], 0.0, ["halo_s"])
            WDT = wsm_bf[:].rearrange("p (k j) -> p k j", j=36)

            MODE = {"rec": rec}
            first_seen = set()
            RINGS = {"F": (0, RING_F), "M": (RING_F, RING_M)}
            ws = {k: {"next_load": 0, "next_use": 0, "released": set()} for k in RINGS}
            PIECE_COLS = {}
            for _p in range(NPIECE):
                PIECE_COLS[_p] = 3072 if 6 <= _p < 14 else 4096

            def slot_of(st, si):
                base, n = RINGS[st]
                return base + si % n

            def emit_load(st, si):
                piece = seq[st][si]
                slot = slot_of(st, si)
                ncol = PIECE_COLS[piece]
                nb = ncol // 2048 if ncol % 2048 == 0 else ncol // 1024
                bsz = ncol // nb
                if piece not in first_seen:
                    first_seen.add(piece)
                    dma("pool", ring[slot][:, 0:ncol].rearrange("p (a b) -> p a b", b=bsz),
                        wall_d[piece][:, 0:ncol].rearrange("p (a b) -> p a b", b=bsz), (), [("ring", slot)],
                        ("wc", slot))
                    dma("sp", scr_d[piece][:, 0:ncol], ring[slot][:, 0:ncol], [("ring", slot)], [("scr", piece)],
                        ("ws", slot))
                else:
                    dma("sp", ring[slot][:, 0:ncol], scr_d[piece][:, 0:ncol], [("scr", piece)], [("ring", slot)],
                        ("wl", slot))

            def pump(st):
                w = ws[st]
                n_slots = RINGS[st][1]
                while w["next_load"] < len(seq[st]):
                    n = w["next_load"]
                    if n >= n_slots and (n - n_slots) not in w["released"]:
                        break
                    emit_load(st, n)
                    w["next_load"] += 1

            def acquire(expect, st):
                if MODE["rec"]:
                    seq[st].append(expect)
                    si = len(seq[st]) - 1
                    sl = slot_of(st, si)
                    return ring[sl], ("ring", sl), (st, si)
                w = ws[st]
                si = w["next_use"]
                assert seq[st][si] == expect, (st, si, seq[st][si], expect)
                pump(st)
                assert w["next_load"] > si, ("weight ring over-subscribed", st, si, expect)
                w["next_use"] += 1
                sl = slot_of(st, si)
                return ring[sl], ("ring", sl), (st, si)

            def release(h):
                if MODE["rec"]:
                    return
                st, si = h
                ws[st]["released"].add(si)
                pump(st)

            def front_norm(blk, nt):
                for t in range(nt):
                    xs = xst[0]
                    xk_ = ("xst", 0)
                    if blk == 0:
                        memset("pool", xs[:], 0.0, [xk_])
                        dma("sp", xs[112:128, :], meta_d, (), [xk_], ("xl", 0))
                    else:
                        r0 = (blk - 1) * 512 + t * 128
                        dma("sp", xs[:], x_d[r0:r0 + 128, :], (), [xk_], ("xl", 0))
                    act(ub[:], xs[:], AF.Square, [xk_], ["ub", "ss"], accum=sm[:, 200:201])
                    act(sm[:, 204:205], sm[:, 200:201], AF.Ln, ["ss"], ["lnss"], bias=EPS, scale=1.0 / D)
                    act(sm[:, 208:209], sm[:, 204:205], AF.Exp, ["lnss"], ["rss"], scale=-0.5)
                    ts("dve", dgF[:], I_f, sm[:, 208:209], None, ALU.mult, None, ["cst", "rss"], ["dgF"])
                    for cg in range(2):
                        bi = newbank()
                        for c4 in range(4):
                            c = cg * 4 + c4
                            mm(pbank[bi][:, c4 * 128:(c4 + 1) * 128], xs[:, c * 128:(c + 1) * 128], dgF[:], True, True,
                               [xk_, "dgF"], [("ps", bi)])
                        tt("dve", uT[:, cg * 4:cg * 4 + 4, t * 128:(t + 1) * 128],
                           v3(pbank[bi][:, 0:512], 128), b3(GMIX[:, cg * 4:cg * 4 + 4], 128), ALU.mult,
                           [("ps", bi), "vpp"], [("uT", t, cg)])
                        relb(bi)
                    yield

            def back_load_x(blk):
                for t in range(4):
                    r0 = (blk - 1) * 512 + t * 128
                    dma("sp", hres[:, t, :], x_d[r0:r0 + 128, :], (), [("hres", t)], ("xh", t))

            def back_norm(nt):
                for t in range(nt):
                    act(ub2[:], hres[:, t, :], AF.Square, [("hres", t)], ["ub2", ("ss2", t)], accum=sm2[:, t:t + 1])
                act(sm2[:, 4:4 + nt], sm2[:, 0:nt], AF.Ln, [("ss2", t) for t in range(nt)], ["lnss2"],
                    bias=EPS, scale=1.0 / D)
                act(sm2[:, 8:8 + nt], sm2[:, 4:4 + nt], AF.Exp, ["lnss2"], ["rss2"], scale=-0.5)
                for t in range(nt):
                    ts("dve", dgM[:], I_f, sm2[:, 8 + t:9 + t], None, ALU.mult, None, ["cst", "rss2"], ["dgM"])
                    for cg in range(2):
                        bi = newbank()
                        for c4 in range(4):
                            c = cg * 4 + c4
                            mm(pbank[bi][:, c4 * 128:(c4 + 1) * 128], hres[:, t, c * 128:(c + 1) * 128], dgM[:], True, True,
                               [("hres", t), "dgM"], [("ps", bi)])
                        tt("dve", u2T[:, cg * 4:cg * 4 + 4, t * 128:(t + 1) * 128],
                           v3(pbank[bi][:, 0:512], 128), b3(GFFN[:, cg * 4:cg * 4 + 4], 128), ALU.mult,
                           [("ps", bi), "vpp"], [("u2T", t, cg)])
                        relb(bi)

            def u2T_keys(nt):
                return [("u2T", t, cg) for t in range(nt) for cg in range(2)]

            def uT_keys(nt):
                return [("uT", t, cg) for t in range(nt) for cg in range(2)]

            def inproj_z(nt):
                for half in range(2):
                    slot, skey, si = acquire(half, "F")
                    wv = slot[:].rearrange("p (k c) -> p k c", c=512)
                    for t in range(nt):
                        bi = newbank()
                        for k in range(8):
                            mm(pbank[bi][:, 0:512], uT[:, k, t * 128:(t + 1) * 128], wv[:, k, :], k == 0, k == 7,
                               [("uT", t, 0), ("uT", t, 1), skey], [("ps", bi)])
                        act(sz[:, t, half * 512:(half + 1) * 512], pbank[bi][:, 0:512], AF.Silu, [("ps", bi)],
                            [("sz", t, half)])
                        relb(bi)
                        yield
                    release(si)

            def inproj_dt(nt):
                for t in range(nt):
                    bi = newbank()
                    for k in range(8):
                        mm(pbank[bi][:, 0:16], uT[:, k, t * 128:(t + 1) * 128], WDT[:, k, 0:16], k == 0, k == 7,
                           [("uT", t, 0), ("uT", t, 1), "wsm_bf"], [("ps", bi)])
                    cp("dve", dtraw[:, t, :], pbank[bi][:, 0:16], [("ps", bi)], [("dtraw", t)])
                    relb(bi)

            def inproj_xbc(nt):
                W = nt * 128
                ukeys = uT_keys(nt)
                for i in range(4):
                    slot, skey, si = acquire(2 + i, "F")
                    wv = slot[:].rearrange("p (k c) -> p k c", c=512)
                    for cc in range(4):
                        ch = 4 * i + cc
                        bi = newbank()
                        for k in range(8):
                            mm(pbank[bi][:, 0:W], wv[:, k, cc * 128:(cc + 1) * 128], uT[:, k, 0:W], k == 0, k == 7,
                               ukeys + [skey], [("ps", bi)])
                        rb = raw[0]
                        rk = ("raw", 0)
                        ak = ("cacc", 0)
                        ab = cacc[0]
                        cp("pool", rb[:, 0:3], halo_x[:, ch, :], ["halo_x"], [rk])
                        cp("act", rb[:, 3:3 + W], pbank[bi][:, 0:W], [("ps", bi)], [rk])
                        relb(bi)
                        ts("pool", ab[:, 0:W], rb[:, 0:W], CW[:, ch * 4:ch * 4 + 1], None, ALU.mult, None,
                           [rk, "vpp"], [ak])
                        stt("dve", ab[:, 0:W], rb[:, 1:W + 1], CW[:, ch * 4 + 1:ch * 4 + 2], ab[:, 0:W], ALU.mult, ALU.add,
                            [rk, ak, "vpp"], [ak])
                        stt("dve", ab[:, 0:W], rb[:, 2:W + 2], CW[:, ch * 4 + 2:ch * 4 + 3], ab[:, 0:W], ALU.mult, ALU.add,
                            [rk, ak, "vpp"], [ak])
                        stt("dve", ab[:, 0:W], rb[:, 3:W + 3], CW[:, ch * 4 + 3:ch * 4 + 4], ab[:, 0:W], ALU.mult, ALU.add,
                            [rk, ak, "vpp"], [ak])
                        cp("pool", halo_x[:, ch, :], rb[:, W:W + 3], [rk], ["halo_x"])
                        act(xbcT[:, ch, 0:W], ab[:, 0:W], AF.Silu, [ak, "vpp"], [("xbcT", ch)], bias=CB[:, ch:ch + 1])
                        yield
                    release(si)

            def inproj_sc(nt, full):
                W = nt * 128
                ukeys = uT_keys(nt)
                names = ["C", "h"] + (["B"] if full else [])

                def tail(ch):
                    gv = xD[:, (ch % 2) * 512:(ch % 2) * 512 + 512]
                    gk = ("xD", ch % 2)
                    sq = yn[:, (ch % 2) * 512:(ch % 2) * 512 + 512]
                    qk = ("yn", ch % 2)
                    bi = newbank()
                    mm(pbank[bi][:, 0:W], BLK, sq[:, 0:W], True, True, [qk] + CBF, [("ps", bi)])
                    act(pbank[bi][:, 0:W], pbank[bi][:, 0:W], AF.Ln, [("ps", bi)], [("ps", bi)], bias=EPS, scale=1.0 / 64)
                    act(pbank[bi][:, 0:W], pbank[bi][:, 0:W], AF.Exp, [("ps", bi)], [("ps", bi)], scale=-0.5)
                    stt("dve", ycatT[:, 8 + ch, 0:W], gv[:, 0:W], GSC[:, ch:ch + 1], pbank[bi][:, 0:W], ALU.mult, ALU.mult,
                        [gk, ("ps", bi), "vpp"], [("ycatT", 8 + ch)])
                    relb(bi)

                pend = None
                for ch in range(8):
                    slot, skey, si = acquire(6 + ch, "F")
                    wv = slot[:, 0:3072].rearrange("p (k c) -> p k c", c=384)
                    banks = {}
                    for nm in names:
                        j = {"B": 0, "C": 1, "h": 2}[nm]
                        bi = newbank()
                        banks[nm] = bi
                        for k in range(8):
                            mm(pbank[bi][:, 0:W], wv[:, k, j * 128:(j + 1) * 128], uT[:, k, 0:W], k == 0, k == 7,
                               ukeys + [skey], [("ps", bi)])
                    release(si)
                    cp("pool", rawsc[:, 0:2], halo_s[:, ch, :], ["halo_s"], ["rawsc"])
                    cp("act", csb[:, 0:W], pbank[banks["C"]][:, 0:W], [("ps", banks["C"])], ["csb"])
                    tt("dve", rawsc[:, 2:2 + W], csb[:, 0:W], pbank[banks["h"]][:, 0:W], ALU.mult,
                       ["csb", ("ps", banks["h"])], ["rawsc"])
                    relb(banks["C"], banks["h"])
                    if full:
                        cp("act", bsb[:, 0:W], pbank[banks["B"]][:, 0:W], [("ps", banks["B"])], ["bsb"])
                        relb(banks["B"])
                        ts("pool", vsc[:, 0:W], rawsc[:, 0:W], SCW[:, ch * 3:ch * 3 + 1], None, ALU.mult, None,
                           ["rawsc", "vpp"], ["vsc"])
                        stt("dve", vsc[:, 0:W], rawsc[:, 1:W + 1], SCW[:, ch * 3 + 1:ch * 3 + 2], vsc[:, 0:W],
                            ALU.mult, ALU.add, ["rawsc", "vsc", "vpp"], ["vsc"])
                        stt("dve", vsc[:, 0:W], rawsc[:, 2:W + 2], SCW[:, ch * 3 + 2:ch * 3 + 3], vsc[:, 0:W],
                            ALU.mult, ALU.add, ["rawsc", "vsc", "vpp"], ["vsc"])
                    cp("pool", halo_s[:, ch, :], rawsc[:, W:W + 2], ["rawsc"], ["halo_s"])
                    if full:
                        gv = xD[:, (ch % 2) * 512:(ch % 2) * 512 + 512]
                        gk = ("xD", ch % 2)
                        sq = yn[:, (ch % 2) * 512:(ch % 2) * 512 + 512]
                        qk = ("yn", ch % 2)
                        tt("pool", gv[:, 0:W], vsc[:, 0:W], bsb[:, 0:W], ALU.mult, ["vsc", "bsb"], [gk])
                        act(sq[:, 0:W], gv[:, 0:W], AF.Square, [gk], [qk])
                        if pend is not None:
                            tail(pend)
                        pend = ch
                    yield
                if pend is not None:
                    tail(pend)

            def ssd_chunk(blk, t, need_y, prev_tail=None, holder=None):
                tsl = slice(t * 128, (t + 1) * 128)
                par = t % 2
                xk, bk = ("xtok", par), ("btok", par)
                for cg in range(2):
                    bi = newbank()
                    for c4 in range(4):
                        c = cg * 4 + c4
                        mm(pbank[bi][:, c4 * 128:(c4 + 1) * 128], xbcT[:, c, tsl], I_b, True, True,
                           [("xbcT", c)] + CBF, [("ps", bi)])
                    cp("act" if cg == 0 else "dve", xtok[par][:, cg * 512:(cg + 1) * 512], pbank[bi][:, 0:512],
                       [("ps", bi)], [xk])
                    relb(bi)
                bi = newbank()
                for c4 in range(4):
                    mm(pbank[bi][:, c4 * 128:(c4 + 1) * 128], xbcT[:, 8 + c4, tsl], I_b, True, True,
                       [("xbcT", 8 + c4)] + CBF, [("ps", bi)])
                cp("act", btok[par][:, 0:512], pbank[bi][:, 0:512], [("ps", bi)], [bk])
                relb(bi)
                yield
                XB, M_, NA, E1, L1, DT, DA = (sm[:, 0:16], sm[:, 16:32], sm[:, 32:48], sm[:, 48:64], sm[:, 64:80],
                                              sm[:, 80:96], sm[:, 96:112])
                tt("dve", XB, dtraw[:, t, :], DTB, ALU.add, [("dtraw", t), "vbc"], ["xb"])
                ts("dve", M_, XB, 0.0, None, ALU.max, None, ["xb"], ["m_"])
                stt("dve", NA, M_, -2.0, XB, ALU.mult, ALU.add, ["xb", "m_"], ["na"])
                act(E1, NA, AF.Exp, ["na"], ["e1"])
                act(L1, E1, AF.Ln, ["e1"], ["l1"], bias=1.0)
                tt("dve", DT, M_, L1, ALU.add, ["m_", "l1"], ["dt"])
                if blk == 0:
                    ts("dve", DT, DT, PADM, None, ALU.mult, None, ["dt", "cst"], ["dt"])
                tt("dve", DA, DT, A_bc[:], ALU.mult, ["dt", "A_bc"], ["da"])
                cbk = newbank()
                mm(pbank[cbk][:, 0:16], TRI, DA, True, True, ["da", "cst"], [("ps", cbk)])
                mm(pbank[cbk][:, 16:32], ONES, DA, True, True, ["da", "cst"], [("ps", cbk)])
                mm(pbank[cbk][0:16, 128:256], DA, TRI, True, True, ["da", "cst"], [("ps", cbk)])
                ACUM, DOUT, CD, TMP, DS, DTDS = (sm[:, 112:128], sm[:, 128:144], sm[:, 144:160], sm[:, 160:176],
                                                 sm[:, 176:192], sm[:, 224:240])
                cp("dve", ACUM, pbank[cbk][:, 0:16], [("ps", cbk)], ["acum"])
                act(CD, pbank[cbk][:, 16:32], AF.Exp, [("ps", cbk)], ["cd"])
                tt("dve", TMP, pbank[cbk][:, 16:32], ACUM, ALU.subtract, [("ps", cbk), "acum"], ["tmp"])
                act(DS, TMP, AF.Exp, ["tmp"], ["ds"])
                tt("dve", DTDS, DT, DS, ALU.mult, ["dt", "ds"], ["dtds"])
                tt("pool", v3(xdd[:], 64), v3(xtok[par][:], 64), b3(DTDS, 64), ALU.mult, [xk, "dtds"], ["xdd"])
                if not need_y:
                    relb(cbk)
                if need_y:
                    act(DOUT, pbank[cbk][:, 0:16], AF.Exp, [("ps", cbk)], ["dout"])
                    cp("act", acT[:], pbank[cbk][0:16, 128:256], [("ps", cbk)], ["acT"])
                    ts("dve", nacT[:], pbank[cbk][0:16, 128:256], -1.0, None, ALU.mult, None, [("ps", cbk)], ["nacT"])
                    tt("dve", v3(xdt[:], 64), v3(xtok[par][:], 64), b3(DT, 64), ALU.mult, [xk, "dt"], ["xdt"])
                    relb(cbk)
                    yield
                    cbb = newbank()
                    for g in range(4):
                        mm(pbank[cbb][:, g * 128:(g + 1) * 128], xbcT[:, 8 + g, tsl], xbcT[:, 12 + g, tsl], True, True,
                           [("xbcT", 8 + g), ("xbcT", 12 + g)], [("ps", cbb)])
                    ydb = [newbank(), newbank()]
                    for g in range(4):
                        sgb = newbank()
                        for r in range(4):
                            h = 4 * g + r
                            o = pbank[sgb][:, r * 128:(r + 1) * 128]
                            mm(o, selv(h), acT[:], True, False, ["cst", "acT"], [("ps", sgb)])
                            mm(o, nacT[:], selv(h), False, False, ["cst", "nacT"], [("ps", sgb)])
                            mm(o, I_b, MASKN, False, True, CBF, [("ps", sgb)])
                        lt = LTb[g % 2]
                        ltk = ("LT", g % 2)
                        act(lt[:], pbank[sgb][:, 0:512], AF.Exp, [("ps", sgb)], [ltk])
                        relb(sgb)
                        mt = MTb[g]
                        mtk = ("MT", g)
                        tt("dve", v3(mt[:], 128), v3(lt[:], 128),
                           pbank[cbb][:, g * 128:(g + 1) * 128].unsqueeze(1).to_broadcast([128, 4, 128]), ALU.mult,
                           [ltk, ("ps", cbb)], [mtk])
                        for r in range(4):
                            h = 4 * g + r
                            mm(pbank[ydb[h // 8]][:, (h % 8) * 64:(h % 8) * 64 + 64], mt[:, r * 128:(r + 1) * 128],
                               xdt[:, h * 64:(h + 1) * 64], True, True, [mtk, "xdt"], [("ps", ydb[h // 8])])
                        yield
                    relb(cbb)
                    yob = [newbank(), newbank()]
                    for g in range(4):
                        mm(pbank[yob[g // 2]][:, (g % 2) * 256:(g % 2) * 256 + 256], xbcT[:, 12 + g, tsl],
                           state_bf[:, g * 256:(g + 1) * 256], True, True, [("xbcT", 12 + g), "state_bf"],
                           [("ps", yob[g // 2])])
                    for hf in range(2):
                        tt("dve", v3(y1[:, hf * 512:(hf + 1) * 512], 64), v3(pbank[yob[hf]][:, 0:512], 64),
                           b3(DOUT[:, hf * 8:(hf + 1) * 8], 64), ALU.mult, [("ps", yob[hf]), "dout"], [("y1", hf)])
                        tt("dve", y1[:, hf * 512:(hf + 1) * 512], y1[:, hf * 512:(hf + 1) * 512], pbank[ydb[hf]][:, 0:512],
                           ALU.add, [("y1", hf), ("ps", ydb[hf])], [("y1", hf)])
                    relb(*yob)
                    relb(*ydb)
                stb = [newbank(), newbank()]
                for g in range(4):
                    mm(pbank[stb[g // 2]][:, (g % 2) * 256:(g % 2) * 256 + 256], btok[par][:, g * 128:(g + 1) * 128],
                       xdd[:, g * 256:(g + 1) * 256], True, True, [bk, "xdd"], [("ps", stb[g // 2])])
                tt("dve", v3(state[:], 64), v3(state[:], 64), b3(CD, 64), ALU.mult, ["state", "cd"], ["state"])
                for hf in range(2):
                    tt("dve", state[:, hf * 512:(hf + 1) * 512], state[:, hf * 512:(hf + 1) * 512], pbank[stb[hf]][:, 0:512],
                       ALU.add, ["state", ("ps", stb[hf])], ["state"])
                relb(*stb)
                cp("pool", state_bf[:], state[:], ["state"], ["state_bf"])
                yield
                if prev_tail is not None:
                    prev_tail()
                if not need_y:
                    return
                tt("pool", v3(xD[:], 64), v3(xtok[par][:], 64), b3(DSK, 64), ALU.mult, [xk, "vbc"], [("xD", 0), ("xD", 1)])
                tt("pool", y1[:], y1[:], xD[:], ALU.add, [("y1", 0), ("y1", 1), ("xD", 0), ("xD", 1)], [("y1", 0), ("y1", 1)])
                tt("pool", y1[:], y1[:], sz[:, t, :], ALU.mult, [("y1", 0), ("y1", 1), ("sz", t, 0), ("sz", t, 1)], [("y1", 0), ("y1", 1)])
                for g in range(4):
                    act(xD[:, g * 256:(g + 1) * 256], y1[:, g * 256:(g + 1) * 256], AF.Square, [("y1", 0), ("y1", 1), ("xD", g // 2)],
                        [("xD", g // 2), ("ssy", g)], accum=sm[:, 240 + g:241 + g])
                act(sm[:, 244:248], sm[:, 240:244], AF.Ln, [("ssy", g) for g in range(4)], ["lny"], bias=EPS, scale=1.0 / 256)
                act(sm[:, 248:252], sm[:, 244:248], AF.Exp, ["lny"], ["rsy"], scale=-0.5)
                tt("dve", v3(yn[:], 256), v3(y1[:], 256), b3(sm[:, 248:252], 256), ALU.mult, [("y1", 0), ("y1", 1), "rsy"], [("yn", 0), ("yn", 1)])

                def tail():
                    for cg in range(2):
                        bi = newbank()
                        for c4 in range(4):
                            c = cg * 4 + c4
                            mm(pbank[bi][:, c4 * 128:(c4 + 1) * 128], yn[:, c * 128:(c + 1) * 128], I_b, True, True,
                               [("yn", cg)] + CBF, [("ps", bi)])
                        tt("dve", ycatT[:, cg * 4:cg * 4 + 4, tsl], v3(pbank[bi][:, 0:512], 128),
                           b3(GSSD[:, cg * 4:cg * 4 + 4], 128), ALU.mult, [("ps", bi), "vpp"], [("ycatT_s", t, cg)])
                        relb(bi)

                holder["tail"] = tail

            def outproj(nt):
                slots = [acquire(14 + i, "M") for i in range(4)]
                for t in range(nt):
                    for half in range(2):
                        bi = newbank()
                        for kc in range(16):
                            slot, skey, si = slots[kc // 4]
                            wv = slot[:].rearrange("p (k c) -> p k c", c=1024)
                            rk = [("ycatT_s", t, kc // 4)] if kc < 8 else [("ycatT", kc)]
                            mm(pbank[bi][:, 0:512], ycatT[:, kc, t * 128:(t + 1) * 128],
                               wv[:, kc % 4, half * 512:(half + 1) * 512], kc == 0, kc == 15, rk + [skey], [("ps", bi)])
                        tt("dve", hres[:, t, half * 512:(half + 1) * 512], hres[:, t, half * 512:(half + 1) * 512],
                           pbank[bi][:, 0:512], ALU.add, [("hres", t), ("ps", bi)], [("hres", t)])
                        relb(bi)
                for s_ in slots:
                    release(s_[2])

            def router_tile(t):
                rt = rt_all[:, t * 160:(t + 1) * 160]
                if True:
                    bi = newbank()
                    for k in range(8):
                        mm(pbank[bi][:, 0:20], u2T[:, k, t * 128:(t + 1) * 128], WDT[:, k, 16:36], k == 0, k == 7,
                           [("u2T", t, 0), ("u2T", t, 1), "wsm_bf"], [("ps", bi)])
                    LG = rt[:, 0:20]
                    GL = rt[:, 0:4]
                    EL = rt[:, 4:20]
                    tt("dve", LG, pbank[bi][:, 0:20], RBIAS, ALU.add, [("ps", bi), "vbc"], [("lg", t)])
                    yield
                    relb(bi)
                    GMAX, NGMAX, GSUM, GVAL = rt[:, 20:21], rt[:, 21:22], rt[:, 22:23], rt[:, 23:24]
                    GM, GE, PEN = rt[:, 24:28], rt[:, 28:32], rt[:, 32:36]
                    redmax(GMAX, GL, [("lg", t)], [("gmax", t)])
                    yield
                    ts("dve", GM, GL, GMAX, None, ALU.is_equal, None, [("lg", t), ("gmax", t)], [("gm", t)])
                    yield
                    ts("dve", NGMAX, GMAX, -1.0, None, ALU.mult, None, [("gmax", t)], [("ngmax", t)])
                    yield
                    act(GE, GL, AF.Exp, [("lg", t), ("ngmax", t)], [("ge", t), ("gsum", t)], bias=NGMAX, scale=1.0, accum=GSUM)
                    yield
                    recip(GVAL, GSUM, [("gsum", t)], [("gval", t)])
                    yield
                    ts("dve", PEN, GM, BIG, -BIG, ALU.mult, ALU.add, [("gm", t)], [("pen", t)])
                    yield
                    ML, MK1, ML2, MK2 = rt[:, 36:52], rt[:, 52:68], rt[:, 68:84], rt[:, 84:100]
                    tt("dve", v3(ML, 4), v3(EL, 4), b3(PEN, 4), ALU.add, [("lg", t), ("pen", t)], [("ml", t)])
                    yield
                    M1, M2, DD, ED, W1, W2 = rt[:, 100:101], rt[:, 101:102], rt[:, 102:103], rt[:, 103:104], \
                        rt[:, 104:105], rt[:, 105:106]
                    redmax(M1, ML, [("ml", t)], [("m1", t)])
                    yield
                    ts("dve", MK1, ML, M1, None, ALU.is_equal, None, [("ml", t), ("m1", t)], [("mk1", t)])
                    yield
                    stt("dve", ML2, MK1, -BIG, ML, ALU.mult, ALU.add, [("mk1", t), ("ml", t)], [("ml2", t)])
                    yield
                    redmax(M2, ML2, [("ml2", t)], [("m2", t)])
                    yield
                    ts("dve", MK2, ML2, M2, None, ALU.is_equal, None, [("ml2", t), ("m2", t)], [("mk2", t)])
                    yield
                    tt("dve", DD, M2, M1, ALU.subtract, [("m1", t), ("m2", t)], [("dd", t)])
                    yield
                    act(ED, DD, AF.Exp, [("dd", t)], [("ed", t)])
                    yield
                    ts("dve", ED, ED, 1.0, None, ALU.add, None, [("ed", t)], [("ed", t)])
                    yield
                    recip(W1, ED, [("ed", t)], [("w1", t)])
                    yield
                    ts("dve", W2, W1, -1.0, 1.0, ALU.mult, ALU.add, [("w1", t)], [("w2", t)])
                    yield
                    C1, C2, COMB = rt[:, 112:128], rt[:, 128:144], rt[:, 144:160]
                    ts("dve", C1, MK1, W1, None, ALU.mult, None, [("mk1", t), ("w1", t)], [("c1", t)])
                    yield
                    stt("dve", C2, MK2, W2, C1, ALU.mult, ALU.add, [("mk2", t), ("w2", t), ("c1", t)], [("c2", t)])
                    yield
                    ts("dve", COMB, C2, GVAL, None, ALU.mult, None, [("c2", t), ("gval", t)], [("comb", t)])
                    yield
                    bj = newbank()
                    mm(pbank[bj][0:16, 0:128], COMB, I_f, True, True, [("comb", t), "cst"], [("ps", bj)])
                    cp("act", combT[:, t * 128:(t + 1) * 128], pbank[bj][0:16, 0:128], [("ps", bj)], [("combT", t)])
                    yield
                    relb(bj)

            def router(nt):
                gens = [router_tile(t) for t in range(nt)]
                alive = [True] * nt
                while any(alive):
                    for t in range(nt):
                        if alive[t]:
                            try:
                                next(gens[t])
                            except StopIteration:
                                alive[t] = False

            def moe(nt):
                W = nt * 128
                ukeys = u2T_keys(nt)
                ctk = [("combT", t) for t in range(nt)]

                def down(e, hb, hk):
                    sd = acquire(20 + 3 * e, "M")
                    wd = sd[0][:].rearrange("p (k c) -> p k c", c=1024)
                    for t in range(nt):
                        for half in range(2):
                            bd = newbank()
                            for j in range(4):
                                mm(pbank[bd][:, 0:512], hb[:, j, t * 128:(t + 1) * 128],
                                   wd[:, j, half * 512:(half + 1) * 512], j == 0, j == 3, [(hk, j), sd[1]], [("ps", bd)])
                            tt("dve", hres[:, t, half * 512:(half + 1) * 512], hres[:, t, half * 512:(half + 1) * 512],
                               pbank[bd][:, 0:512], ALU.add, [("hres", t), ("ps", bd)], [("hres", t)])
                            relb(bd)
                        yield
                    release(sd[2])

                pend = None
                for e in range(16):
                    sg = acquire(18 + 3 * e, "M")
                    su = acquire(19 + 3 * e, "M")
                    wg = sg[0][:].rearrange("p (k c) -> p k c", c=512)
                    wu = su[0][:].rearrange("p (k c) -> p k c", c=512)
                    bi = newbank()
                    mm(pbank[bi][:, 0:W], selv(e), combT[:, 0:W], True, True, ["cst"] + ctk, [("ps", bi)])
                    act(cb_sb[:, 0:W], pbank[bi][:, 0:W], AF.Copy, [("ps", bi)], ["cb_sb"], scale=0.5)
                    relb(bi)
                    hb = hT[e % 2]
                    hk = ("hT", e % 2)
                    for j in range(4):
                        bg = newbank()
                        for k in range(8):
                            mm(pbank[bg][:, 0:W], wg[:, k, j * 128:(j + 1) * 128], u2T[:, k, 0:W], k == 0, k == 7,
                               ukeys + [sg[1]], [("ps", bg)])
                        bu = newbank()
                        for k in range(8):
                            mm(pbank[bu][:, 0:W], wu[:, k, j * 128:(j + 1) * 128], u2T[:, k, 0:W], k == 0, k == 7,
                               ukeys + [su[1]], [("ps", bu)])
                        sbuf_s = s_sb[0]
                        sk = ("s_sb", 0)
                        tb = t_sb[j % 2]
                        tk = ("t_sb", j % 2)
                        act(sbuf_s[:, 0:W], pbank[bg][:, 0:W], AF.Tanh, [("ps", bg)], [sk], scale=0.5)
                        stt("dve", tb[:, 0:W], sbuf_s[:, 0:W], 1.0, pbank[bg][:, 0:W], ALU.add, ALU.mult,
                            [sk, ("ps", bg)], [tk])
                        tt("dve", tb[:, 0:W], tb[:, 0:W], pbank[bu][:, 0:W], ALU.mult, [tk, ("ps", bu)], [tk])
                        relb(bg, bu)
                        tt("pool", hb[:, j, 0:W], tb[:, 0:W], cb_sb[:, 0:W], ALU.mult, [tk, "cb_sb"], [(hk, j)])
                        yield
                    release(sg[2])
                    release(su[2])
                    if pend is not None:
                        yield from down(*pend)
                    pend = (e, hb, hk)
                yield from down(*pend)

            def final_norm_store(blk, nt):
                for t in range(nt):
                    act(ub2[:], hres[:, t, :], AF.Square, [("hres", t)], ["ub2", ("ssf", t)], accum=sm2[:, 12 + t:13 + t])
                act(sm2[:, 16:16 + nt], sm2[:, 12:12 + nt], AF.Ln, [("ssf", t) for t in range(nt)], ["lnf"],
                    bias=EPS, scale=1.0 / D)
                act(sm2[:, 20:20 + nt], sm2[:, 16:16 + nt], AF.Exp, ["lnf"], ["rsf"], scale=-0.5)
                for t in range(nt):
                    stt("dve", hres[:, t, :], hres[:, t, :], sm2[:, 20 + t:21 + t], GFIN, ALU.mult,
                        ALU.mult, [("hres", t), "rsf", "vbc"], [("hres", t)])
                    r0 = (blk - 1) * 512 + t * 128
                    dma("sp", out_d[r0:r0 + 128, :], hres[:, t, :], [("hres", t)], [("out", r0)], ("os", t))


            def drain(g):
                for _ in g:
                    pass

            def front(blk, nt, full):
                yield from front_norm(blk, nt)
                if full:
                    yield from inproj_z(nt)
                inproj_dt(nt)
                yield from inproj_xbc(nt)
                marker("NEED", "outproj_done")
                yield from inproj_sc(nt, full)
                tail_fn = None
                for t in range(nt):
                    holder = {}
                    yield from ssd_chunk(blk, t, full, tail_fn, holder)
                    tail_fn = holder.get("tail")
                if tail_fn is not None:
                    tail_fn()

            def interleave(gm, gf, ratio):
                alive_m, alive_f = True, gf is not None
                while alive_m or alive_f:
                    for _ in range(ratio):
                        if alive_m:
                            try:
                                next(gm)
                            except StopIteration:
                                alive_m = False
                    if alive_f:
                        try:
                            next(gf)
                        except StopIteration:
                            alive_f = False

            def record(stream, fn):
                REC["list"] = []
                STREAM["cur"] = stream
                fn()
                lst = REC["list"]
                REC["list"] = None
                STREAM["cur"] = "F"
                return lst

            def merge(lM, lF):
                out = []
                idx = {"M": 0, "F": 0}
                lists = {"M": lM, "F": lF}
                n = {"M": max(1, len(lM)), "F": max(1, len(lF))}
                marks = set()
                eng_free = {e: 0.0 for e in ENGS}
                wr = {}
                rd = {}

                def avail(st):
                    i = idx[st]
                    if i >= len(lists[st]):
                        return False
                    it = lists[st][i]
                    if it[0] == "NEED" and it[1] not in marks:
                        return False
                    return True

                def est(item):
                    if item[0] in ("MARK", "NEED"):
                        return -1.0
                    eng = item[0]
                    t = eng_free[eng]
                    for k in item[2]:
                        w = wr.get(k)
                        if w is not None:
                            t = max(t, w[0] + (XLAT if w[1] != eng else 0.0))
                    for k in item[3]:
                        w = wr.get(k)
                        if w is not None:
                            t = max(t, w[0] + (XLAT if w[1] != eng else 0.0))
                        r = rd.get(k)
                        if r is not None:
                            t = max(t, r + XLAT)
                    return t

                while idx["M"] < len(lM) or idx["F"] < len(lF):
                    aM, aF = avail("M"), avail("F")
                    assert aM or aF, "unsatisfied cross-stream marker"
                    if aM and aF:
                        tM = est(lM[idx["M"]])
                        tF = est(lF[idx["F"]])
                        if abs(tM - tF) < 1e-9:
                            pick = "F" if idx["F"] / n["F"] <= idx["M"] / n["M"] else "M"
                        else:
                            pick = "M" if tM < tF else "F"
                    else:
                        pick = "M" if aM else "F"
                    item = lists[pick][idx[pick]]
                    idx[pick] += 1
                    if item[0] == "MARK":
                        marks.add(item[1])
                        continue
                    if item[0] == "NEED":
                        continue
                    start = est(item)
                    eng, cost = item[0], item[5]
                    if item[4] is not None:
                        eng_free[eng] = start + 0.06
                        end = start + cost
                    else:
                        end = start + cost
                        eng_free[eng] = end
                    for k in item[2]:
                        rd[k] = max(rd.get(k, 0.0), end)
                    for k in item[3]:
                        wr[k] = (end, eng)
                        rd[k] = 0.0
                    out.append(item[:5])
                return out

            def back(blk):
                back_load_x(blk)
                outproj(4)
                marker("MARK", "outproj_done")
                back_norm(4)
                router(4)
                drain(moe(4))
                final_norm_store(blk, 4)

            def schedule(ops, sid):
                n_ = len(ops)
                preds = [set() for _ in range(n_)]
                last_w = {}
                readers = {}
                for i, op in enumerate(ops):
                    for k in op[2]:
                        w = last_w.get(k)
                        if w is not None:
                            preds[i].add(w)
                    for k in op[3]:
                        w = last_w.get(k)
                        if w is not None:
                            preds[i].add(w)
                        for r in readers.get(k, ()):
                            preds[i].add(r)
                    for k in op[2]:
                        readers.setdefault(k, []).append(i)
                    for k in op[3]:
                        last_w[k] = i
                        readers[k] = []
                    preds[i].discard(i)
                succs = [[] for _ in range(n_)]
                indeg = [0] * n_
                for i in range(n_):
                    indeg[i] = len(preds[i])
                    for p in preds[i]:
                        succs[p].append(i)
                eng_free = {e: 0.0 for e in ENGS}
                fin = [0.0] * n_
                rdy = [0.0] * n_
                ready = set(i for i in range(n_) if indeg[i] == 0)
                order = []
                pos = [0] * n_
                cnt_s = {}
                for i in range(n_):
                    pos[i] = cnt_s.get(sid[i], 0)
                    cnt_s[sid[i]] = pos[i] + 1
                done_s = {k: [False] * v for k, v in cnt_s.items()}
                oldest = {k: 0 for k in cnt_s}
                while ready:
                    best = None
                    bt = None
                    for i in ready:
                        if pos[i] > oldest[sid[i]] + LOOKAHEAD:
                            continue
                        t = max(eng_free[ops[i][0]], rdy[i])
                        if bt is None or t < bt - 1e-9 or (abs(t - bt) <= 1e-9 and i < best):
                            best, bt = i, t
                    ready.discard(best)
                    st_ = sid[best]
                    done_s[st_][pos[best]] = True
                    while oldest[st_] < len(done_s[st_]) and done_s[st_][oldest[st_]]:
                        oldest[st_] += 1
                    op = ops[best]
                    eng, cost = op[0], op[5]
                    if op[4] is not None:
                        eng_free[eng] = bt + 0.06
                    else:
                        eng_free[eng] = bt + cost
                    fin[best] = bt + cost
                    order.append(best)
                    for s_ in succs[best]:
                        lat = XLAT if (ops[s_][0] != eng or op[4] is not None) else 0.0
                        if fin[best] + lat > rdy[s_]:
                            rdy[s_] = fin[best] + lat
                        indeg[s_] -= 1
                        if indeg[s_] == 0:
                            ready.add(s_)
                assert len(order) == n_
                return [ops[i][:5] for i in order]

            def window(lM, lF):
                cut = 0
                for i, it in enumerate(lM):
                    if it[0] == "MARK":
                        cut = i
                        break
                base = [(it, "M") for it in lM[:cut]] + [(it, "F") for it in lF] + [(it, "M") for it in lM[cut:]]
                base = [b_ for b_ in base if b_[0][0] not in ("MARK", "NEED")]
                return [b_[0] for b_ in base], [b_[1] for b_ in base]

            l0 = record("F", lambda: drain(front(0, 1, False)))
            l1 = record("F", lambda: drain(front(1, 4, True)))
            for item in schedule(*window([], l0 + l1)):
                SS[0].add(*item)
            for blk in range(1, 9):
                lM = record("M", lambda: back(blk))
                lF = record("F", lambda: drain(front(blk + 1, 4, True))) if blk < 8 else []
                for item in schedule(*window(lM, lF)):
                    SS[0].add(*item)
            SS[0].add("sp", None, reads=[("out", r) for r in range(0, SEQ, 128)])

        seq = {"F": [], "M": []}
        body(True, seq)
        print("sbuf bytes remaining", nc.sbuf_bytes_remaining)
        SS[0] = Sched()
        body(False, seq)
        SS[0].emit(nc)
    return nc


def _piece(w, rows_per_chunk_cols):
    K, C = w.shape
    nk = K // 128
    return np.ascontiguousarray(w.reshape(nk, 128, C).transpose(1, 0, 2).reshape(128, nk * C))


def _host_layout(inputs):
    f = lambda a: np.asarray(a, dtype=np.float32)
    w_in = f(inputs["w_in"])[0]
    w_out = f(inputs["w_out"])[0]
    wg = f(inputs["expert_w_gate"])[0]
    wu = f(inputs["expert_w_up"])[0]
    wd = f(inputs["expert_w_down"])[0]
    wall = np.zeros((NPIECE, 128, 4096), np.float32)
    wall[0] = _piece(w_in[:, 0:512], None)
    wall[1] = _piece(w_in[:, 512:1024], None)
    for i in range(4):
        wall[2 + i] = _piece(w_in[:, 1024 + 512 * i:1024 + 512 * (i + 1)], None)
    o = 3088
    for ch in range(8):
        cols = np.concatenate([w_in[:, o + 1024 * j + 128 * ch:o + 1024 * j + 128 * (ch + 1)] for j in range(3)], axis=1)
        wall[6 + ch][:, 0:3072] = _piece(cols, None)
    for i in range(4):
        wall[14 + i] = _piece(w_out[512 * i:512 * (i + 1), :], None)
    for e in range(16):
        wall[18 + 3 * e] = _piece(wg[e], None)
        wall[19 + 3 * e] = _piece(wu[e], None)
        wall[20 + 3 * e] = _piece(wd[e], None)
    wsmall_src = np.concatenate([w_in[:, 3072:3088], f(inputs["router_group_w"])[0], f(inputs["router_expert_w"])[0]],
                                axis=1)
    wsmall = _piece(wsmall_src, None)
    vrow = np.concatenate([f(inputs["norm_final"]), f(inputs["dt_bias"])[0], f(inputs["A_log"])[0],
                           f(inputs["D_skip"])[0], f(inputs["router_group_b"])[0], f(inputs["router_expert_b"])[0]])
    vbc = np.ascontiguousarray(np.broadcast_to(vrow[None, :], (128, vrow.shape[0])))
    pp = lambda v: np.ascontiguousarray(v.reshape(-1, 128).T)
    cw = f(inputs["ssd_conv_w"])[0]
    cwpp = np.ascontiguousarray(cw.reshape(4, 16, 128).transpose(2, 1, 0).reshape(128, 64))
    scw = f(inputs["sc_conv_w"])[0]
    scwpp = np.ascontiguousarray(scw.reshape(3, 8, 128).transpose(2, 1, 0).reshape(128, 24))
    vpp = np.concatenate([pp(f(inputs["norm_mix"])[0]), pp(f(inputs["norm_ffn"])[0]), pp(f(inputs["ssd_norm"])[0]),
                          pp(f(inputs["sc_norm"])[0]), cwpp, pp(f(inputs["ssd_conv_b"])[0]), scwpp], axis=1)
    vpp = np.ascontiguousarray(vpp.astype(np.float32))
    cst = np.zeros((128, 641), np.float32)
    cst[:, 0:128] = np.eye(128)
    cst[:, 128:256] = np.triu(np.ones((128, 128)))
    cst[:, 256:384] = 1.0
    cst[:, 384:512] = np.where(np.arange(128)[None, :] >= np.arange(128)[:, None], 0.0, -BIG)
    blk = np.zeros((128, 128), np.float32)
    blk[0:64, 0:64] = 1.0
    blk[64:128, 64:128] = 1.0
    cst[:, 512:640] = blk
    cst[112:128, 640] = 1.0
    shared = {"meta": f(inputs["meta_tokens"]), "wall": wall, "wsmall": wsmall, "vbc": vbc, "vpp": vpp, "cst": cst}
    return shared


_NC_CACHE = {}


def kernel(**inputs):
    x = np.asarray(inputs["x"], dtype=np.float32)
    shared = _host_layout(inputs)
    if "nc" not in _NC_CACHE:
        _NC_CACHE["nc"] = build_program()
    nc = _NC_CACHE["nc"]
    in_maps = []
    for c in range(NCORES):
        m = dict(shared)
        m["x"] = np.ascontiguousarray(x[c])
        in_maps.append(m)
    res = run_bass_kernel_spmd(nc, in_maps, core_ids=list(range(NCORES)))
    out = np.stack([np.asarray(r["out"], dtype=np.float32) for r in res.results], axis=0)
    if DEBUG is not None:
        kernel.dbg = [r.get("dbg") for r in res.results]
    return out
```
